# Optimizing a Trainium2 kernel written in Bass

```python
import math
import numpy as np
import jax
import jax.numpy as jnp
from jax import lax

D_MODEL = 1024
BATCH = 2
SEQ = 8192
DEPTH = 4

D_MIX = D_MODEL
SSD_WIDTH = D_MIX // 2
SSD_HEAD_DIM = 64
SSD_HEADS = SSD_WIDTH // SSD_HEAD_DIM
SSD_GROUPS = 2
SSD_STATE = 64
SSD_CONV = 4
SSD_CONV_DIM = SSD_WIDTH + 2 * SSD_GROUPS * SSD_STATE
SSD_CHUNK = 128
DT_MIN = 0.001
DT_MAX = 0.1
HGRN_WIDTH = D_MIX // 4
HGRN_HEAD_DIM = 64
HGRN_HEADS = HGRN_WIDTH // HGRN_HEAD_DIM
HGRN_CHUNK = 16
GATE_FLOOR = 1e-30
RET_WIDTH = D_MIX // 4
RET_HEAD_DIM = 64
RET_HEADS = RET_WIDTH // RET_HEAD_DIM
RET_CHUNK = 128
ROPE_BASE = 10000.0
IN_WIDTHS = (SSD_WIDTH, SSD_CONV_DIM, SSD_HEADS,
             HGRN_WIDTH, HGRN_WIDTH, HGRN_WIDTH, HGRN_WIDTH,
             RET_WIDTH, RET_WIDTH, RET_WIDTH, RET_WIDTH)
D_IN = SSD_WIDTH + SSD_CONV_DIM + SSD_HEADS + 4 * HGRN_WIDTH + 4 * RET_WIDTH
N_GROUPS = 4
EXPERTS_PER_GROUP = 8
N_EXPERTS = N_GROUPS * EXPERTS_PER_GROUP
TOP_K = 2
D_EXPERT = D_MODEL // 4
ADA_SCALE = 0.5
EPS = 1e-6

kernel_name = 'hybrid_ssd_hgrn2_retention_hmoe_adaln'


def rms(x):
    xf = x.astype(jnp.float32)
    return xf * lax.rsqrt(jnp.mean(xf * xf, axis=-1, keepdims=True) + EPS)


def rmsnorm(x, g):
    return rms(x) * g


def causal_depthwise_conv(x, w, bias):
    width, ch = w.shape
    out = lax.conv_general_dilated(
        x, w[:, None, :].astype(x.dtype), window_strides=(1,), padding=[(width - 1, 0)],
        dimension_numbers=('NWC', 'WIO', 'NWC'), feature_group_count=ch)
    return out + bias


def rotary(x, pos):
    half = x.shape[-1] // 2
    inv_freq = ROPE_BASE ** (-jnp.arange(half, dtype=jnp.float32) / half)
    ang = pos[:, None] * inv_freq[None, :]
    cos = jnp.cos(ang)[None, :, None, :]
    sin = jnp.sin(ang)[None, :, None, :]
    x1 = x[..., :half].astype(jnp.float32)
    x2 = x[..., half:].astype(jnp.float32)
    return jnp.concatenate([x1 * cos - x2 * sin, x1 * sin + x2 * cos], axis=-1)


def chunked_linear_recurrence(q, k, v, log_a, chunk):
    b, l, h, dk = q.shape
    dv = v.shape[-1]
    nc = l // chunk
    q, k, v, log_a = (t.astype(jnp.float32).reshape(b, nc, chunk, h, t.shape[-1])
                      for t in (q, k, v, log_a))
    cum = jnp.cumsum(log_a, axis=2)
    causal = jnp.tril(jnp.ones((chunk, chunk), dtype=bool))
    if log_a.shape[-1] == 1:
        cs = jnp.moveaxis(cum[..., 0], 2, 3)
        diff = cs[..., :, None] - cs[..., None, :]
        decay = jnp.where(causal, jnp.exp(jnp.where(causal, diff, 0.0)), 0.0)
        scores = jnp.einsum('bcthk,bcshk->bchts', q, k) * decay
    else:
        diff = cum[:, :, :, None] - cum[:, :, None]
        m = causal[:, :, None, None]
        decay = jnp.where(m, jnp.exp(jnp.where(m, diff, 0.0)), 0.0)
        scores = jnp.einsum('bcthk,bcshk,bctshk->bchts', q, k, decay)
    o_intra = jnp.einsum('bchts,bcshv->bcthv', scores, v)
    k_end = k * jnp.exp(cum[:, :, -1:] - cum)
    chunk_states = jnp.einsum('bcshk,bcshv->bchkv', k_end, v)
    chunk_decay = jnp.exp(cum[:, :, -1])

    def carry_state(s, inp):
        a, s_c = inp
        return a[..., None] * s + s_c, s

    _, s_prev = lax.scan(carry_state, jnp.zeros((b, h, dk, dv), jnp.float32),
                         (jnp.moveaxis(chunk_decay, 1, 0), jnp.moveaxis(chunk_states, 1, 0)))
    s_prev = jnp.moveaxis(s_prev, 0, 1)
    o_inter = jnp.einsum('bcthk,bchkv->bcthv', q * jnp.exp(cum), s_prev)
    return (o_intra + o_inter).reshape(b, l, h, dv)


def token_mixer(h, w_in, conv_w, conv_b, dt_bias, a_log, d_skip, ssd_norm_g,
                lower_bound, hgrn_norm_g, w_out):
    b, l, _ = h.shape
    proj = h @ w_in
    offsets = np.cumsum(IN_WIDTHS)[:-1].tolist()
    z, xbc, dt, hq, hf, hi, hg, rq, rk, rv, rg = jnp.split(proj, offsets, axis=-1)

    def heads(t, n):
        return t.reshape(b, l, n, -1)

    xbc = jax.nn.silu(causal_depthwise_conv(xbc, conv_w, conv_b))
    xs, bm, cm = jnp.split(xbc, [SSD_WIDTH, SSD_WIDTH + SSD_GROUPS * SSD_STATE], axis=-1)
    rep = SSD_HEADS // SSD_GROUPS
    xs = heads(xs, SSD_HEADS)
    bm = jnp.repeat(heads(bm, SSD_GROUPS), rep, axis=2)
    cm = jnp.repeat(heads(cm, SSD_GROUPS), rep, axis=2)
    dt = jax.nn.softplus(dt.astype(jnp.float32) + dt_bias)
    log_a = (dt * -jnp.exp(a_log.astype(jnp.float32)))[..., None]
    y = chunked_linear_recurrence(cm, bm, xs * dt[..., None], log_a, SSD_CHUNK)
    y = y + d_skip[:, None] * xs
    y = y * jax.nn.silu(heads(z, SSD_HEADS).astype(jnp.float32))
    y_ssd = rmsnorm(y.reshape(b, l, SSD_GROUPS, -1),
                    ssd_norm_g.reshape(SSD_GROUPS, -1)).reshape(b, l, SSD_WIDTH)

    raw = hf.astype(jnp.float32)
    forget = lower_bound + (1.0 - lower_bound) * jax.nn.sigmoid(raw)
    log_f = jnp.log(jnp.maximum(forget, GATE_FLOOR))
    k_in = 1.0 - forget
    o = chunked_linear_recurrence(heads(hq, HGRN_HEADS), heads(k_in, HGRN_HEADS),
                                  heads(hi, HGRN_HEADS), heads(log_f, HGRN_HEADS), HGRN_CHUNK)
    o = rmsnorm(o, hgrn_norm_g.reshape(HGRN_HEADS, HGRN_HEAD_DIM))
    o_hgrn = (o * jax.nn.sigmoid(heads(hg, HGRN_HEADS).astype(jnp.float32))).reshape(b, l, HGRN_WIDTH)

    pos = jnp.arange(l, dtype=jnp.float32)
    q_r = rotary(heads(rq, RET_HEADS), pos)
    k_r = rotary(heads(rk, RET_HEADS), pos) * RET_HEAD_DIM ** -0.5
    log_gamma = jnp.log1p(-jnp.exp2(-5.0 - jnp.arange(RET_HEADS, dtype=jnp.float32)))
    log_g = jnp.broadcast_to(log_gamma[:, None], (b, l, RET_HEADS, 1))
    o = chunked_linear_recurrence(q_r, k_r, heads(rv, RET_HEADS), log_g, RET_CHUNK)
    o_ret = (rms(o) * jax.nn.silu(heads(rg, RET_HEADS).astype(jnp.float32))).reshape(b, l, RET_WIDTH)

    mixed = jnp.concatenate([y_ssd, o_hgrn, o_ret], axis=-1).astype(h.dtype)
    return mixed @ w_out


def hierarchical_moe(h, w_grp, b_grp, w_exp, b_exp, w_gate, w_up, w_down):
    b, l, d = h.shape
    t = h.reshape(b * l, d)
    grp_logits = (t @ w_grp + b_grp).astype(jnp.float32)
    grp_prob = jax.nn.softmax(grp_logits, axis=-1)
    g_idx = jnp.argmax(grp_logits, axis=-1)
    p_grp = jnp.max(grp_prob, axis=-1, keepdims=True)
    exp_logits = (t @ w_exp + b_exp).astype(jnp.float32).reshape(-1, N_GROUPS, EXPERTS_PER_GROUP)
    in_grp = jnp.einsum('tg,tge->te', jax.nn.one_hot(g_idx, N_GROUPS, dtype=jnp.float32), exp_logits)
    top_logit, top_idx = lax.top_k(in_grp, TOP_K)
    weights = jax.nn.softmax(top_logit, axis=-1) * p_grp
    expert_id = g_idx[:, None] * EXPERTS_PER_GROUP + top_idx
    combine = jnp.einsum('tk,tke->te', weights,
                         jax.nn.one_hot(expert_id, N_EXPERTS, dtype=jnp.float32)).astype(t.dtype)
    y = jnp.zeros_like(t)
    for gi in range(N_GROUPS):
        sl = slice(gi * EXPERTS_PER_GROUP, (gi + 1) * EXPERTS_PER_GROUP)
        hid = jax.nn.silu(jnp.einsum('td,edf->tef', t, w_gate[sl])) * jnp.einsum('td,edf->tef', t, w_up[sl])
        y = y + jnp.einsum('tef,efd->td', hid * combine[:, sl, None], w_down[sl])
    return y.reshape(b, l, d)


def setup_inputs(seed: int = 0) -> dict:
    key = jax.random.key(seed)
    ks = jax.random.split(key, 24)
    f32 = jnp.float32

    def nrm(k, shape, scale):
        return scale * jax.random.normal(k, shape, f32)

    dt0 = jnp.exp(jax.random.uniform(ks[9], (DEPTH, SSD_HEADS), f32, math.log(DT_MIN), math.log(DT_MAX)))
    return {
        'x': nrm(ks[0], (BATCH, SEQ, D_MODEL), 1.0),
        'c': nrm(ks[1], (BATCH, D_MODEL), 1.0),
        'norm_mix_g': 1.0 + nrm(ks[2], (DEPTH, D_MODEL), 0.02),
        'norm_ffn_g': 1.0 + nrm(ks[3], (DEPTH, D_MODEL), 0.02),
        'final_norm_g': 1.0 + nrm(ks[4], (D_MODEL,), 0.02),
        'w_ada': nrm(ks[5], (DEPTH, D_MODEL, 6 * D_MODEL), ADA_SCALE * D_MODEL ** -0.5),
        'b_ada': nrm(ks[6], (DEPTH, 6 * D_MODEL), 0.02),
        'w_in': nrm(ks[7], (DEPTH, D_MODEL, D_IN), D_MODEL ** -0.5),
        'conv_w': nrm(ks[8], (DEPTH, SSD_CONV, SSD_CONV_DIM), SSD_CONV ** -0.5),
        'conv_b': nrm(ks[10], (DEPTH, SSD_CONV_DIM), 0.02),
        'ssd_dt_bias': dt0 + jnp.log(-jnp.expm1(-dt0)),
        'ssd_a_log': jnp.log(jax.random.uniform(ks[11], (DEPTH, SSD_HEADS), f32, 1.0, 16.0)),
        'ssd_d': 1.0 + nrm(ks[12], (DEPTH, SSD_HEADS), 0.1),
        'ssd_norm_g': 1.0 + nrm(ks[13], (DEPTH, SSD_WIDTH), 0.02),
        'hgrn_lower_bounds': nrm(ks[14], (DEPTH, HGRN_WIDTH), 0.1),
        'hgrn_norm_g': 1.0 + nrm(ks[15], (DEPTH, HGRN_WIDTH), 0.02),
        'w_out': nrm(ks[16], (DEPTH, D_MIX, D_MODEL), D_MIX ** -0.5),
        'w_grp': nrm(ks[17], (DEPTH, D_MODEL, N_GROUPS), D_MODEL ** -0.5),
        'b_grp': nrm(ks[18], (DEPTH, N_GROUPS), 0.01),
        'w_exp': nrm(ks[19], (DEPTH, D_MODEL, N_EXPERTS), D_MODEL ** -0.5),
        'b_exp': nrm(ks[20], (DEPTH, N_EXPERTS), 0.01),
        'w_gate': nrm(ks[21], (DEPTH, N_EXPERTS, D_MODEL, D_EXPERT), D_MODEL ** -0.5),
        'w_up': nrm(ks[22], (DEPTH, N_EXPERTS, D_MODEL, D_EXPERT), D_MODEL ** -0.5),
        'w_down': nrm(ks[23], (DEPTH, N_EXPERTS, D_EXPERT, D_MODEL), D_EXPERT ** -0.5),
    }


def reference(x, c, norm_mix_g, norm_ffn_g, final_norm_g, w_ada, b_ada, w_in, conv_w, conv_b,
              ssd_dt_bias, ssd_a_log, ssd_d, ssd_norm_g, hgrn_lower_bounds, hgrn_norm_g, w_out,
              w_grp, b_grp, w_exp, b_exp, w_gate, w_up, w_down):
    lb_soft = jax.nn.softmax(hgrn_lower_bounds.astype(jnp.float32), axis=0)
    lower_bound = jnp.cumsum(lb_soft, axis=0) - lb_soft[0]
    c_act = jax.nn.silu(c)
    for layer in range(DEPTH):
        mod = c_act @ w_ada[layer] + b_ada[layer]
        sh_mix, sc_mix, g_mix, sh_ffn, sc_ffn, g_ffn = jnp.split(mod[:, None, :], 6, axis=-1)
        h = (rmsnorm(x, norm_mix_g[layer]) * (1.0 + sc_mix) + sh_mix).astype(x.dtype)
        mixed = token_mixer(h, w_in[layer], conv_w[layer], conv_b[layer], ssd_dt_bias[layer],
                            ssd_a_log[layer], ssd_d[layer], ssd_norm_g[layer], lower_bound[layer],
                            hgrn_norm_g[layer], w_out[layer])
        x = x + (g_mix * mixed).astype(x.dtype)
        h = (rmsnorm(x, norm_ffn_g[layer]) * (1.0 + sc_ffn) + sh_ffn).astype(x.dtype)
        moe = hierarchical_moe(h, w_grp[layer], b_grp[layer], w_exp[layer], b_exp[layer],
                               w_gate[layer], w_up[layer], w_down[layer])
        x = x + (g_ffn * moe).astype(x.dtype)
    return rmsnorm(x, final_norm_g).astype(x.dtype)
```

```python
import contextlib
import math
import numpy as np
import concourse.bass as bass
import concourse.mybir as mybir
from concourse.bass_utils import run_bass_kernel_spmd

F32 = mybir.dt.float32
BF16 = mybir.dt.bfloat16
AF = mybir.ActivationFunctionType
ALU = mybir.AluOpType
AX = mybir.AxisListType

EPOCH = 4000
D = 1024
NT = 16
TB = 2
BT = 128 * TB
NBLK = NT // TB
NCORE = 8
SEG = 2048
DEPTH = 4
EPS = 1e-6
NEG = -1.0e30
CUT = 99
CUT2 = 99

G_XS, G_BCF, G_RK, G_HIRV, G_DT, G_HQRQ, G_RQS, G_Z, G_HGRG = range(9)
GCOLS = [512, 512, 512, 512, 8, 512, 256, 512, 512]
GOFF = [0]
for _c in GCOLS:
    GOFF.append(GOFF[-1] + _c)
DINP = GOFF[-1]


class Buf:
    __slots__ = ("name", "w", "r", "excl")

    def __init__(self, name):
        self.name = name
        self.w = None
        self.r = []
        self.excl = False


class Op:
    __slots__ = ("eng", "idx", "fn", "deps", "dma", "done", "inc")

    def __init__(self, eng, idx, fn, deps, dma):
        self.eng = eng
        self.idx = idx
        self.fn = fn
        self.deps = deps
        self.dma = dma
        self.done = None
        self.inc = False


class Sched:
    ENGS = ("pe", "act", "dve", "pool", "sp")

    def __init__(self, nc, n_dma_sems=40, same_eng_sync=True):
        self.nc = nc
        self.ops = {e: [] for e in self.ENGS}
        self.same = same_eng_sync
        self.n_dma = n_dma_sems
        self.dma_uses = [0] * n_dma_sems
        self.dma_last = [None] * n_dma_sems
        n_sw = n_dma_sems // 2
        self.dma_ring = {"pool": (0, n_sw), "sp": (n_sw, n_dma_sems - n_sw), "act": (n_sw, n_dma_sems - n_sw)}
        self.dma_rr = {"pool": 0, "sp": 0, "act": 0}
        self.nbuf = 0

    def buf(self, name=None):
        self.nbuf += 1
        return Buf(name or f"b{self.nbuf}")

    def bufs(self, n, name="b"):
        return [self.buf(f"{name}{i}") for i in range(n)]

    def _deps(self, reads, writes):
        deps = []
        for b in reads:
            if b.w is not None:
                deps.append(b.w)
        for b in writes:
            if b.w is not None:
                deps.append(b.w)
            deps.extend(b.r)
        return deps

    def _commit(self, op, reads, writes):
        for b in reads:
            b.r.append(op)
        for b in writes:
            b.w = op
            b.r = []

    def op(self, eng, fn, reads=(), writes=()):
        if any(b.excl for b in reads):
            writes = list(writes) + [b for b in reads if b.excl and b not in writes]
            reads = [b for b in reads if not b.excl]
        deps = self._deps(reads, writes)
        o = Op(eng, len(self.ops[eng]), fn, deps, None)
        self.ops[eng].append(o)
        self._commit(o, reads, writes)
        return o

    def dma(self, eng, fn, reads=(), writes=()):
        deps = self._deps(reads, writes)
        base, n = self.dma_ring[eng]
        key = "sp" if eng == "act" else eng
        r = base + self.dma_rr[key]
        self.dma_rr[key] = (self.dma_rr[key] + 1) % n
        if self.dma_last[r] is not None:
            deps.append(self.dma_last[r])
        self.dma_uses[r] += 1
        o = Op(eng, len(self.ops[eng]), fn, deps, (r, 16 * self.dma_uses[r]))
        self.dma_last[r] = o
        self.ops[eng].append(o)
        self._commit(o, reads, writes)
        return o

    def _skip(self, d, e):
        return d.eng == e and (e == "pe" or e == "sp" or not self.same)

    def emit(self, final_waits=()):
        nc = self.nc
        for e in self.ENGS:
            for o in self.ops[e]:
                for d in o.deps:
                    if d.dma is None and not self._skip(d, e):
                        d.inc = True
        for o in final_waits:
            if o.dma is None:
                o.inc = True
        nsem = {}
        for e in self.ENGS:
            c = 0
            for o in self.ops[e]:
                if o.dma is None and o.inc:
                    o.done = (e, c // EPOCH, c % EPOCH + 1)
                    c += 1
            nsem[e] = (c + EPOCH - 1) // EPOCH
        with contextlib.ExitStack() as st:
            esems = {e: [st.enter_context(nc.semaphore(f"s_{e}{i}")) for i in range(nsem[e])]
                     for e in self.ENGS}
            dsems = [st.enter_context(nc.semaphore(f"s_dma{i}")) for i in range(self.n_dma)]
            block = st.enter_context(nc.Block())

            def run(e, eng):
                seen = {}
                for o in self.ops[e]:
                    for d in o.deps:
                        if d.dma is not None:
                            key = ("d", d.dma[0]); sem = dsems[d.dma[0]]; val = d.dma[1]
                        else:
                            if self._skip(d, e):
                                continue
                            key = (d.done[0], d.done[1]); sem = esems[d.done[0]][d.done[1]]; val = d.done[2]
                        if seen.get(key, 0) >= val:
                            continue
                        seen[key] = val
                        eng.wait_ge(sem, val)
                    ins = o.fn(eng)
                    if o.dma is not None:
                        ins.then_inc(dsems[o.dma[0]], 16)
                    elif o.inc:
                        ins.then_inc(esems[e][o.done[1]], 1)
                if e == "sp":
                    for o in final_waits:
                        if o.dma is not None:
                            eng.wait_ge(dsems[o.dma[0]], o.dma[1])
                        else:
                            eng.wait_ge(esems[o.done[0]][o.done[1]], o.done[2])

            @block.tensor
            def _(eng):
                run("pe", eng)

            @block.scalar
            def _(eng):
                run("act", eng)

            @block.vector
            def _(eng):
                run("dve", eng)

            @block.gpsimd
            def _(eng):
                run("pool", eng)

            @block.sync
            def _(eng):
                run("sp", eng)


def _consts(seg):
    c = {}
    p = np.arange(128)
    t = np.arange(128)
    c["ident"] = np.eye(128, dtype=np.float32)
    c["negmask"] = np.where(p[:, None] <= t[None, :], 0.0, NEG).astype(np.float32)
    c["blk32"] = ((p[:, None] <= t[None, :]) & (p[:, None] // 32 == t[None, :] // 32)).astype(np.float32)
    gam = 1.0 - 2.0 ** (-5.0 - np.arange(4))
    lg = np.log1p(-(2.0 ** (-5.0 - np.arange(4, dtype=np.float64))))
    dm = np.zeros((128, 4, 128), np.float32)
    for h in range(4):
        dm[:, h, :] = np.where(p[:, None] <= t[None, :], 0.125 * np.exp(lg[h] * (t[None, :] - p[:, None])), 0.0)
    c["retmask"] = dm
    gq = np.zeros((128, 2, 128), np.float32)
    g128 = np.zeros((128, 2), np.float32)
    g2048 = np.zeros((128, 2), np.float32)
    for cp in range(2):
        for hh in range(2):
            h = cp * 2 + hh
            gq[hh * 64:(hh + 1) * 64, cp, :] = np.exp(lg[h] * (t + 1))[None, :]
            g128[hh * 64:(hh + 1) * 64, cp] = np.exp(lg[h] * 128)
            g2048[hh * 64:(hh + 1) * 64, cp] = np.exp(lg[h] * SEG)
    c["gq"] = gq
    c["g128"] = g128
    c["g2048"] = g2048
    gk = np.zeros((128, 4), np.float32)
    for h in range(4):
        gk[:, h] = 0.125 * np.exp(lg[h] * (127 - p))
    c["gk"] = gk
    r128 = np.ones((128, BT), np.float32); r128[:, ::128] = 0.0
    r32 = np.ones((128, BT), np.float32); r32[:, ::32] = 0.0
    c["reset128"] = r128
    c["reset32"] = r32
    c["mask96"] = (p >= 96).astype(np.float32)[:, None]
    f = p % 64
    invf = (10000.0 ** (-(f % 32).astype(np.float32) / np.float32(32))).astype(np.float32)
    pos = (seg * SEG + np.arange(SEG)).astype(np.float32)
    ang = (pos[None, :] * invf[:, None]).astype(np.float32)
    sgn = np.where(f < 32, -1.0, 1.0).astype(np.float32)
    c["rcos"] = np.cos(ang).astype(np.float32)
    c["rsin"] = (np.sin(ang) * sgn[:, None]).astype(np.float32)
    return c


CONST_SHAPES = {
    "ident": [128, 128], "negmask": [128, 128], "blk32": [128, 128], "retmask": [128, 4, 128],
    "gq": [128, 2, 128], "g128": [128, 2], "g2048": [128, 2], "gk": [128, 4],
    "reset128": [128, BT], "reset32": [128, BT],
    "mask96": [128, 1], "rcos": [128, SEG], "rsin": [128, SEG],
}

NPM = 54
NBC = 812


def build(n_layers=DEPTH, do_final=True, dbg=False, stage=9, fake_cc=False):
    nc = bass.Bass("TRN2", target_bir_lowering=False)

    def din(name, shape):
        return nc.dram_tensor(name, shape, F32, kind="ExternalInput").ap()

    x_d = din("x", [SEG, D])
    cT_d = din("cT", [128, 8])
    pm_d = din("pm", [DEPTH, NPM, 128])
    bc_d = din("bcrow", [DEPTH, NBC])
    dtp_d = din("dtp", [DEPTH, 8, 2])
    fng_d = din("final_g", [1, D])
    wada_d = din("w_ada", [DEPTH, D, 6 * D])
    bada_d = din("b_ada", [DEPTH, 6 * D])
    win_d = din("w_in", [DEPTH, D, DINP])
    wout_d = din("w_out", [DEPTH, D, D])
    wr_d = din("w_r", [DEPTH, D, 36])
    _ed = (DEPTH if n_layers > 1 else 1, 32) if stage >= 6 else (1, 1)
    wg_d = din("w_gate", [_ed[0], _ed[1], D, 256])
    wu_d = din("w_up", [_ed[0], _ed[1], D, 256])
    wd_d = din("w_down", [_ed[0], _ed[1], 256, D])
    segm_d = din("segmask", [128, 4])
    prev_d = din("prevsel", [128, 4])
    cst_d = {k: din("c_" + k, v) for k, v in CONST_SHAPES.items()}
    out_d = nc.dram_tensor("out", [SEG, D], F32, kind="ExternalOutput").ap()
    NS = 1088

    win_bf = [nc.dram_tensor(f"win_bf{l}", [D, DINP], BF16) for l in range(DEPTH)]
    S = Sched(nc)
    b_winbf = S.bufs(DEPTH, "winbf")
    st = contextlib.ExitStack()
    with st:
        def sb(name, shape, dt=F32):
            return st.enter_context(nc.sbuf_tensor("s_" + name, shape, dt))

        def ps(name, shape, dt=F32):
            return st.enter_context(nc.psum_tensor("p_" + name, shape, dt))

        ccsem = st.enter_context(nc.semaphore("ccsem"))
        cc_count = [0]

        def MM(out, lhsT, rhs, start, stop, R, W):
            return S.op("pe", lambda e: e.matmul(out, lhsT=lhsT, rhs=rhs, start=start, stop=stop), R, W)

        def TR(out, in_, idn, R, W):
            return S.op("pe", lambda e: e.transpose(out, in_, idn), R, W)

        def ACT(out, in_, func, R, W, bias=None, scale=None, accum=None):
            kw = {}
            if bias is not None:
                kw["bias"] = bias
            if scale is not None:
                kw["scale"] = scale
            if accum is not None:
                kw["accum_out"] = accum
            return S.op("act", lambda e: e.activation(out=out, in_=in_, func=func, **kw), R, W)

        def TS(eng, out, in0, s1, s2, op0, op1, R, W):
            if op1 is None:
                return S.op(eng, lambda e: e.tensor_scalar(out=out, in0=in0, scalar1=s1, scalar2=None, op0=op0), R, W)
            return S.op(eng, lambda e: e.tensor_scalar(out=out, in0=in0, scalar1=s1, scalar2=s2, op0=op0, op1=op1), R, W)

        def TT(eng, out, in0, in1, op, R, W):
            return S.op(eng, lambda e: e.tensor_tensor(out=out, in0=in0, in1=in1, op=op), R, W)

        def STT(out, in0, scalar, in1, op0, op1, R, W):
            return S.op("dve", lambda e: e.scalar_tensor_tensor(out=out, in0=in0, scalar=scalar, in1=in1, op0=op0, op1=op1), R, W)

        def CP(eng, out, in_, R, W):
            if eng == "act":
                return S.op(eng, lambda e: e.activation(out=out, in_=in_, func=AF.Copy), R, W)
            return S.op(eng, lambda e: e.tensor_copy(out=out, in_=in_), R, W)

        def MS(eng, out, val, W):
            return S.op(eng, lambda e: e.memset(out, val), (), W)

        def DMA(q, out, in_, R, W):
            return S.dma(q, lambda e: e.dma_start(out=out, in_=in_), R, W)

        def RED(out, in_, op, R, W):
            return S.op("dve", lambda e: e.tensor_reduce(out=out, in_=in_, axis=AX.X, op=op), R, W)

        def RECIP(out, in_, R, W):
            return S.op("dve", lambda e: e.reciprocal(out=out, in_=in_), R, W)

        def SCAN(out, d0, d1, R, W):
            return S.op("dve", lambda e: e.tensor_tensor_scan(out=out, data0=d0, data1=d1, initial=0.0,
                                                              op0=ALU.mult, op1=ALU.add), R, W)

        def allgather(src_sb, width, R, W_rcv):
            i = cc_count[0]
            cc_count[0] += 1
            b_in, b_out = S.buf(), S.buf()
            cin_t = nc.dram_tensor(f"ccin_{i}", [128, width], F32)
            cout_t = nc.dram_tensor(f"ccout_{i}", [512, width], F32)
            DMA("sp", cin_t.ap(), src_sb, R, [b_in])
            val = i + 1

            def coll(e):
                ins = e.collective_compute("AllGather", ALU.bypass, replica_groups=[[0, 1, 2, 3], [4, 5, 6, 7]],
                                           ins=[cin_t.ap().opt()], outs=[cout_t.ap().opt()])
                ins.then_inc(ccsem)
                e.wait_ge(ccsem, val)
                return e.memset(ccdummy[:, 0:1], 0.0)
            if fake_cc:
                for r in range(4):
                    DMA("sp", cout_t.ap()[r * 128:(r + 1) * 128, :], cin_t.ap(), [b_in], [b_out])
            else:
                S.op("pool", coll, [b_in], [b_out, b_ccd])
            return cout_t.ap(), b_out

        cst = {}
        cstb = {}
        for k, shp in CONST_SHAPES.items():
            if k in ("rcos", "rsin"):
                continue
            cst[k] = sb("k_" + k, shp)
            cstb[k] = S.buf("k_" + k)
            DMA("sp", cst[k][:], cst_d[k], [], [cstb[k]])
        ccdummy = sb("ccdummy", [128, 4]); b_ccd = S.buf()
        identb = sb("identb", [128, 128], BF16); b_identb = S.buf()
        CP("dve", identb[:], cst["ident"][:], [cstb["ident"]], [b_identb])
        sel8 = sb("sel8", [8, 128], BF16); b_sel8 = S.buf()
        cum3 = sb("cum3", [8, 3, BT], BF16); b_cum3 = S.buf()
        sel32 = sb("sel32", [32, 128], BF16); b_sel32 = S.buf()
        segm = sb("segm", [128, 4]); b_segm = S.buf()
        prevs = sb("prevs", [128, 4]); b_prevs = S.buf()
        DMA("sp", segm[:], segm_d, [], [b_segm])
        DMA("sp", prevs[:], prev_d, [], [b_prevs])

        X = sb("X", [128, NT, D]); bX = S.bufs(NT, "X")
        for i in range(NT):
            DMA("sp", X[:, i, :], x_d[i * 128:(i + 1) * 128, :], [], [bX[i]])
        hTf = sb("hTf", [128, 8, SEG], BF16); b_hTf = S.bufs(4, "hTf")
        hTf_flat = hTf[:].rearrange("p a b -> p (a b)")
        carve_off = [0]
        carved = []

        class View:
            def __init__(self, ap):
                self.ap = ap

            def __getitem__(self, idx):
                return self.ap[idx]

        def carve(shape, b=None):
            n = 1
            for s_ in shape[1:]:
                n *= s_
            ap = hTf_flat[0:shape[0], carve_off[0]:carve_off[0] + n]
            carve_off[0] += n
            assert carve_off[0] <= 8 * SEG
            if len(shape) == 3:
                ap = ap.rearrange("p (a b) -> p a b", b=shape[2])
            elif len(shape) == 4:
                ap = ap.rearrange("p (a b c) -> p a b c", b=shape[2], c=shape[3])
            bb = S.buf()
            carved.append(bb)
            return View(ap), bb

        NWB = 2
        wbuf = [sb(f"wbuf{i}", [128, 8, 512], BF16) for i in range(NWB)]; b_wbuf = S.bufs(NWB, "wbuf")
        wrr = [0]
        wbig = sb("wbig", [128, 8192], BF16); b_wgu = S.bufs(2, "wgu")
        wgu = [View(wbig[:, k * 4096:(k + 1) * 4096].rearrange("p (kc c) -> p kc c", c=512)) for k in range(2)]
        wdn = [sb(f"wdn{i}", [128, 2, D], BF16) for i in range(2)]; b_wdn = S.bufs(2, "wdn")
        wout = View(wbig[:, :].rearrange("p (kc c) -> p kc c", c=D))
        wr = sb("wr", [128, 8, 36], BF16); b_wr = S.buf()

        def load_wgroup(src2d, ncols, q="pool", R=()):
            i = wrr[0] % NWB
            wrr[0] += 1
            DMA(q, wbuf[i][:, :, 0:ncols], src2d.rearrange("(kc p) c -> p kc c", p=128), list(R), [b_wbuf[i]])
            return wbuf[i], b_wbuf[i]

        def convert_win(l):
            for r in range(8):
                DMA("pool", win_bf[l].ap()[r * 128:(r + 1) * 128, :], win_d[l][r * 128:(r + 1) * 128, :], [], [b_winbf[l]])

        def load_win(l, g, ncols=None):
            n = GCOLS[g] if ncols is None else ncols
            return load_wgroup(win_bf[l].ap()[:, GOFF[g]:GOFF[g] + n], n, q="sp", R=[b_winbf[l]])

        pmraw = sb("pmraw", [NPM, 128]); b_pmraw = S.buf()
        pmT = sb("pmT", [128, NPM]); b_pmT = S.buf()
        bcr = sb("bcr", [128, NBC]); b_bcr = S.buf()
        dtp = sb("dtp", [8, 2]); b_dtp = S.buf()
        negA = sb("negA", [8, 1]); b_negA = S.buf()
        lbs = sb("lbs", [128, 12]); b_lbs = S.buf()
        cact = sb("cact", [128, 8]); b_cact = S.buf()
        cab = sb("cab", [128, 8, 128], BF16); b_cab = S.buf()
        modT = sb("modT", [128, 32]); b_modT = S.buf()
        s1 = sb("s1", [128, 16]); b_s1 = S.buf()
        gbc = sb("gbc", [128, 2, D]); b_gbc = S.bufs(2, "gbc")

        pA = [ps(f"pA{i}", [128, 512]) for i in range(2)]; b_pA = S.bufs(2, "pA"); pArr = [0]
        pT = [ps(f"pT{i}", [128, 512]) for i in range(2)]; b_pT = S.bufs(2, "pT"); pTrr = [0]
        pTRb = ps("pTRb", [128, 1024], BF16); b_pTR = S.buf()
        pSC = ps("pSC", [128, 512]); b_pSC = S.buf()
        pO = ps("pO", [128, 1024]); b_pO = S.buf()

        for _b in list(b_pA) + list(b_pT) + [b_pTR, b_pSC, b_pO]:
            _b.excl = True

        def nextA():
            i = pArr[0] % 2
            pArr[0] += 1
            return pA[i], b_pA[i]

        def nextT():
            i = pTrr[0] % 2
            pTrr[0] += 1
            return pT[i], b_pT[i]

        DMA("sp", cact[:], cT_d, [], [b_cact])
        ACT(cact[:], cact[:], AF.Silu, [b_cact], [b_cact])
        CP("dve", cab[:], cact[:].unsqueeze(2).broadcast_to([128, 8, 128]), [b_cact], [b_cab])

        ssq = sb("ssq", [128, 8]); b_ssq = S.buf()
        xn = sb("xn", [128, D], BF16); b_xn = S.buf()
        hT, b_hT = carve([128, 8, BT])
        rawx = sb("rawx", [128, 6, 3 + BT]); b_rawx = S.buf()
        ctmp = sb("ctmp", [128, BT]); b_ctmp = S.buf()
        xbc, b_xbc = carve([128, 6, BT])
        dtf = sb("dtf", [8, 3, BT]); b_dtf = S.buf()
        dttok = sb("dttok", [128, TB, 40]); b_dttok = S.buf()
        ecr = sb("ecr", [128, BT]); b_ecr = S.buf()
        _Cpj = [carve([128, 128]) for _ in range(2)]; Cpj = [a for a, _ in _Cpj]; b_Cpj = [b for _, b in _Cpj]
        dendb = sb("dendb", [128, TB, 8]); b_dendb = S.buf()
        dm = sb("dm", [128, 128]); b_dm = S.buf()
        _sT = [carve([128, 128]) for i in range(2)]; sT = [a for a, _ in _sT]; b_sT = [b for _, b in _sT]; sTrr = [0]
        xs1, b_xs1 = carve([128, 512])
        xs2, b_xs2 = carve([128, 512])
        xsd = sb("xsd", [128, 512]); b_xsd = S.buf()
        Btok, b_Btok = carve([128, 128])
        Sssd = sb("Sssd", [128, 512]); b_Sssd = S.buf()
        Sssdb, b_Sssdb = carve([128, 512])
        Dtot = sb("Dtot", [128, 8]); b_Dtot = S.buf()
        hsg = sb("hsg", [128, 2, BT]); b_hsg = S.buf()
        hlf = sb("hlf", [128, 2, BT]); b_hlf = S.buf()
        hcum = sb("hcum", [128, 2, BT]); b_hcum = S.buf()
        hrc = sb("hrc", [128, 2, BT]); b_hrc = S.buf()
        cres = View(hrc[0:8, :, :]); b_cres = b_hrc
        kend, b_kend = carve([128, 2, BT])
        qp, b_qp = carve([128, 2, BT])
        qpm, b_qpm = carve([128, 2, TB, 64])
        kp, b_kp = carve([128, 2, BT])
        ecend = sb("ecend", [128, 2, BT // 32]); b_ecend = S.buf()
        Lsum = sb("Lsum", [128, 4]); b_Lsum = S.buf()
        kendt, b_kendt = carve([128, 256])
        kendm, b_kendm = carve([128, 256])
        _hv = [carve([128, 256]) for _ in range(TB)]; hvs = [a for a, _ in _hv]; b_hvs = [b for _, b in _hv]
        Sh = [sb(f"Sh{i}", [128, 128]) for i in range(2)]; b_Sh = S.bufs(2, "Sh")
        _Shs = [[carve([128, 128]) for sc in range(4)] for c in range(2)]
        Shs = [[a for a, _ in row] for row in _Shs]; b_Shs = [[b for _, b in row] for row in _Shs]
        rtab = sb("rtab", [128, 2, BT]); b_rtab = S.buf()
        rt1 = sb("rt1", [128, BT]); b_rt1 = S.buf()
        rt2 = sb("rt2", [128, BT]); b_rt2 = S.buf()
        kr, b_kr = carve([128, 2, BT])
        qr, b_qr = carve([128, 2, BT])
        qrg, b_qrg = carve([128, 2, BT])
        krt, b_krt = carve([128, 256])
        _rv = [carve([128, 256]) for _ in range(TB)]; rvbs = [a for a, _ in _rv]; b_rvbs = [b for _, b in _rv]
        Sr = [sb(f"Sr{i}", [128, 128]) for i in range(2)]; b_Sr = S.bufs(2, "Sr")
        _Srb = [carve([128, 128]) for i in range(2)]; Srb = [a for a, _ in _Srb]; b_Srb = [b for _, b in _Srb]
        gzs = [sb(f"gz{t}", [128, 1088]) for t in range(TB)]; b_gzs = S.bufs(TB, "gz")
        gz, b_gz = gzs[0], b_gzs[0]
        yy = sb("yy", [128, 1088]); b_yy = S.buf()
        nst = sb("nst", [128, 24]); b_nst = S.buf()
        mixed, b_mixed = carve([128, D])
        mT = View(hT.ap[:, :, 0:128]); b_mT = b_hT
        otmp = sb("otmp", [128, 512]); b_otmp = S.buf()
        snd, b_snd = gz, b_gz
        rcv, b_rcv = yy, b_yy
        xt1, b_xt1 = otmp, b_otmp
        halo = sb("halo", [128, 18]); b_halo = S.buf()
        lg_sb = View(ecr[:, 64:100]); b_lg = b_ecr
        rt = View(ecr[:, 0:64]); b_rt = b_ecr
        comb = View(ecr[:, 128:160]); b_comb = b_ecr
        combT = View(gzs[0][0:32, :].bitcast(BF16)[:, 0:SEG]); b_combT = [b_gzs[0]] * 4
        cbs = View(yy[:, 0:512]); b_cbs = S.buf()
        sgs = View(yy[:, 512:1024]); b_sgs = S.buf()
        tmo, b_tmo = otmp, b_otmp
        modg, b_modg = cbs, b_cbs
        badab, b_badab = sgs, b_sgs
        hids = [View(gzs[1][:, :].bitcast(BF16)[:, k * 1024:(k + 1) * 1024].rearrange("p (a b) -> p a b", b=512)) for k in range(2)]
        b_hids = S.bufs(2, "hid")
        hidrr = [0]

        MS("pool", qpm[:], 0.0, [b_qpm])
        gm = sb("gm", [128, 2]); b_gm = S.buf()
        MS("dve", gm[:], 0.0, [b_gm])
        MS("dve", gm[0:64, 0:1], 1.0, [b_gm])
        MS("dve", gm[64:128, 1:2], 1.0, [b_gm])
        _Cm = [carve([128, BT]) for _ in range(2)]; Cm = [a for a, _ in _Cm]; b_Cm = [b for _, b in _Cm]
        sel8all, b_sel8all = carve([8, 8, 128])
        qp0, b_qp0 = carve([128, 2, 128])
        qr0, b_qr0 = carve([128, 2, 128])
        qrg0, b_qrg0 = carve([128, 2, 128])
        qpm0, b_qpm0 = carve([128, 2, 64])

        dbg_outs = []

        def rstd_from_ssq(col, n, scale):
            TS("dve", ssq[:, col:col + n], ssq[:, col:col + n], scale, EPS, ALU.mult, ALU.add, [b_ssq], [b_ssq])
            ACT(ssq[:, col:col + n], ssq[:, col:col + n], AF.Sqrt, [b_ssq], [b_ssq])
            RECIP(ssq[:, col:col + n], ssq[:, col:col + n], [b_ssq], [b_ssq])

        def norm_to_hT(i, dst, dst_b, toff, s1col, shcol):
            ACT(xn[:], X[:, i, :], AF.Square, [bX[i]], [b_xn, b_ssq], accum=ssq[:, 0:1])
            rstd_from_ssq(0, 1, 1.0 / D)
            TS("dve", xn[:], X[:, i, :], ssq[:, 0:1], None, ALU.mult, None, [bX[i], b_ssq], [b_xn])
            for j in range(8):
                TR(pTRb[:, j * 128:(j + 1) * 128], xn[:, j * 128:(j + 1) * 128], identb[:], [b_xn, b_identb], [b_pTR])
            for j in range(8):
                ACT(dst[:, j, toff:toff + 128], pTRb[:, j * 128:(j + 1) * 128], AF.Identity,
                    [b_pTR, b_s1, b_modT], [dst_b], bias=modT[:, shcol + j:shcol + j + 1], scale=s1[:, s1col + j:s1col + j + 1])

        def layer_params(l):
            DMA("sp", pmraw[:], pm_d[l], [], [b_pmraw])
            pa, bpa = nextA()
            TR(pa[:, 0:NPM], pmraw[:], cst["ident"][0:NPM, 0:NPM], [b_pmraw, cstb["ident"]], [bpa])
            CP("dve", pmT[:], pa[:, 0:NPM], [bpa], [b_pmT])
            DMA("sp", bcr[:], bc_d[l:l + 1, :].broadcast_to([128, NBC]), [], [b_bcr])
            DMA("sp", dtp[:], dtp_d[l], [], [b_dtp])
            ACT(negA[:], dtp[:, 1:2], AF.Exp, [b_dtp], [b_negA])
            TS("dve", negA[:], negA[:], -1.0, None, ALU.mult, None, [b_negA], [b_negA])
            ACT(lbs[:, 4:12], pmT[:, 46:54], AF.Exp, [b_pmT], [b_lbs])
            for c in range(2):
                TT("dve", lbs[:, 2 + c:3 + c], lbs[:, 4 + c:5 + c], lbs[:, 6 + c:7 + c], ALU.add, [b_lbs], [b_lbs])
                TT("dve", lbs[:, 2 + c:3 + c], lbs[:, 2 + c:3 + c], lbs[:, 8 + c:9 + c], ALU.add, [b_lbs], [b_lbs])
                TT("dve", lbs[:, 2 + c:3 + c], lbs[:, 2 + c:3 + c], lbs[:, 10 + c:11 + c], ALU.add, [b_lbs], [b_lbs])
                RECIP(lbs[:, 2 + c:3 + c], lbs[:, 2 + c:3 + c], [b_lbs], [b_lbs])
                MS("dve", lbs[:, c:c + 1], 0.0, [b_lbs])
                for lp in range(1, l + 1):
                    TT("dve", lbs[:, c:c + 1], lbs[:, c:c + 1], lbs[:, 4 + 2 * lp + c:5 + 2 * lp + c], ALU.add, [b_lbs], [b_lbs])
                TT("dve", lbs[:, c:c + 1], lbs[:, c:c + 1], lbs[:, 2 + c:3 + c], ALU.mult, [b_lbs], [b_lbs])
                TS("dve", lbs[:, 2 + c:3 + c], lbs[:, c:c + 1], -1.0, 1.0, ALU.mult, ALU.add, [b_lbs], [b_lbs])
            for g in range(12):
                wb, bwb = load_wgroup(wada_d[l][:, g * 512:(g + 1) * 512], 512)
                DMA("sp", badab[:], bada_d[l:l + 1, g * 512:(g + 1) * 512].broadcast_to([128, 512]), [], [b_badab])
                pa, bpa = nextA()
                for kc in range(8):
                    MM(pa[:], cab[:, kc, :], wb[:, kc, :], kc == 0, kc == 7, [b_cab, bwb], [bpa])
                which = g // 2
                half = g % 2
                if which in (2, 5):
                    gi = 0 if which == 2 else 1
                    TT("dve", gbc[:, gi, half * 512:(half + 1) * 512], pa[:], badab[:], ALU.add, [bpa, b_badab], [b_gbc[gi]])
                else:
                    TT("dve", modg[:], pa[:], badab[:], ALU.add, [bpa, b_badab], [b_modg])
                    base = {0: 0, 1: 8, 3: 16, 4: 24}[which] + half * 4
                    pt_, bpt_ = nextT()
                    for j in range(4):
                        TR(pt_[:, j * 128:(j + 1) * 128], modg[:, j * 128:(j + 1) * 128], cst["ident"][:], [b_modg, cstb["ident"]], [bpt_])
                    CP("dve", modT[:, base:base + 4], pt_[:, 0:512:128], [bpt_], [b_modT])
            TS("dve", s1[:, 0:8], modT[:, 8:16], 1.0, None, ALU.add, None, [b_modT], [b_s1])
            TT("dve", s1[:, 0:8], s1[:, 0:8], pmT[:, 0:8], ALU.mult, [b_s1, b_pmT], [b_s1])
            TS("dve", s1[:, 8:16], modT[:, 24:32], 1.0, None, ALU.add, None, [b_modT], [b_s1])
            TT("dve", s1[:, 8:16], s1[:, 8:16], pmT[:, 8:16], ALU.mult, [b_s1, b_pmT], [b_s1])

        def inproj_F(l, g, chunks, consume):
            wb, bwb = load_win(l, g)
            for ci in chunks:
                pa, bpa = nextA()
                for kc in range(8):
                    MM(pa[:, 0:BT], wb[:, kc, ci * 128:(ci + 1) * 128], hT[:, kc, :], kc == 0, kc == 7, [bwb, b_hT], [bpa])
                consume(ci, pa, bpa)

        def inproj_T(l, g, consume):
            wb, bwb = load_win(l, g)
            for t in range(TB):
                pt_, bpt_ = nextT()
                for kc in range(8):
                    MM(pt_[:, 0:GCOLS[g]], hT[:, kc, t * 128:(t + 1) * 128], wb[:, kc, 0:GCOLS[g]], kc == 0, kc == 7, [bwb, b_hT], [bpt_])
                consume(t, pt_, bpt_)

        def mixer_block(l, blk, full):
            t0 = blk * TB
            for t in range(TB):
                norm_to_hT(t0 + t, hT, b_hT, t * 128, 0, 0)
            if blk > 0:
                CP("pool", rawx[:, :, 0:3], rawx[:, :, BT:BT + 3], [b_rawx], [b_rawx])
            else:
                CP("pool", rawx[:, :, 0:3], halo[:].rearrange("p (c k) -> p c k", k=3), [b_halo, b_rawx], [b_rawx])

            def conv_chunk(c, pa, bpa):
                ACT(rawx[:, c, 3:3 + BT], pa[:, 0:BT], AF.Copy, [bpa], [b_rawx])
                TS("dve", ctmp[:], rawx[:, c, 0:BT], pmT[:, 16 + c * 4:17 + c * 4], pmT[:, 40 + c:41 + c], ALU.mult, ALU.add,
                   [b_rawx, b_pmT], [b_ctmp])
                for j in range(1, 4):
                    STT(ctmp[:], rawx[:, c, j:j + BT], pmT[:, 16 + c * 4 + j:17 + c * 4 + j], ctmp[:], ALU.mult, ALU.add,
                        [b_rawx, b_pmT, b_ctmp], [b_ctmp])
                ACT(xbc[:, c, :], ctmp[:], AF.Silu, [b_ctmp], [b_xbc])

            inproj_F(l, G_XS, range(4), conv_chunk)
            if CUT <= 1:
                return

            def bcf_chunk(ci, pa, bpa):
                if ci < 2:
                    conv_chunk(4 + ci, pa, bpa)
                else:
                    c = ci - 2
                    ACT(hsg[:, c, :], pa[:, 0:BT], AF.Sigmoid, [bpa], [b_hsg])
            inproj_F(l, G_BCF, range(4), bcf_chunk)

            for c in range(2):
                TS("dve", hsg[:, c, :], hsg[:, c, :], lbs[:, 2 + c:3 + c], lbs[:, c:c + 1], ALU.mult, ALU.add, [b_hsg, b_lbs], [b_hsg])
                TS("dve", hlf[:, c, :], hsg[:, c, :], 1e-30, None, ALU.max, None, [b_hsg], [b_hlf])
            ACT(hlf[:], hlf[:], AF.Ln, [b_hlf], [b_hlf])
            TS("dve", hsg[:], hsg[:], -1.0, 1.0, ALU.mult, ALU.add, [b_hsg], [b_hsg])
            for c in range(2):
                SCAN(hcum[:, c, :], cst["reset32"][:], hlf[:, c, :], [cstb["reset32"], b_hlf], [b_hcum])
            NSUB = BT // 32
            for c in range(2):
                cv = hcum[:, c, :].rearrange("p (s k) -> p s k", k=32)
                TT("dve", hrc[:, c, :].rearrange("p (s k) -> p s k", k=32), cv[:, :, 31:32].broadcast_to([128, NSUB, 32]), cv,
                   ALU.subtract, [b_hcum], [b_hrc])
            ACT(hrc[:], hrc[:], AF.Exp, [b_hrc], [b_hrc])
            TT("dve", kend[:], hsg[:], hrc[:], ALU.mult, [b_hsg, b_hrc], [b_kend])
            for c in range(2):
                ACT(ecend[:, c, :], hcum[:, c, 31:BT:32], AF.Exp, [b_hcum], [b_ecend])
                if not full:
                    RED(Lsum[:, 2 + c:3 + c], hcum[:, c, 31:BT:32], ALU.add, [b_hcum], [b_Lsum])
                    TT("dve", Lsum[:, c:c + 1], Lsum[:, c:c + 1], Lsum[:, 2 + c:3 + c], ALU.add, [b_Lsum], [b_Lsum])

            if CUT <= 2:
                return
            DMA("sp", rtab[:, 0, :], cst_d["rcos"][:, blk * BT:(blk + 1) * BT], [], [b_rtab])
            DMA("sp", rtab[:, 1, :], cst_d["rsin"][:, blk * BT:(blk + 1) * BT], [], [b_rtab])

            def rot_group(dst, dst_b):
                def f(ci, pa, bpa):
                    if ci < 2:
                        TT("dve", (rt1 if ci == 0 else rt2)[:], pa[:, 0:BT], rtab[:, 0, :], ALU.mult, [bpa, b_rtab],
                           [b_rt1 if ci == 0 else b_rt2])
                    else:
                        c = ci - 2
                        src, bsrc = (rt1, b_rt1) if c == 0 else (rt2, b_rt2)
                        STT(dst[:, c, :], pa[:, 0:BT], 1.0, rtab[:, 1, :], ALU.mult, ALU.mult, [bpa, b_rtab], [dst_b])
                        TT("dve", dst[:, c, :], dst[:, c, :], src[:], ALU.add, [dst_b, bsrc], [dst_b])
                return f
            inproj_F(l, G_RK, range(4), rot_group(kr, b_kr))

            def dt_chunk(ci, pa, bpa):
                ACT(dtf[:, 0, :], pa[0:8, 0:BT], AF.Exp, [bpa, b_dtp], [b_dtf], bias=dtp[:, 0:1])
                ACT(dtf[:, 0, :], dtf[:, 0, :], AF.Ln, [b_dtf], [b_dtf], bias=1.0)
                TS("dve", dtf[:, 1, :], dtf[:, 0, :], negA[:, 0:1], None, ALU.mult, None, [b_dtf, b_negA], [b_dtf])
                SCAN(dtf[:, 2, :], cst["reset128"][0:8, :], dtf[:, 1, :], [cstb["reset128"], b_dtf], [b_dtf])
                CP("dve", cum3[:, 0, :], dtf[:, 2, :], [b_dtf], [b_cum3])
                TT("dve", cres[:, 0, :], dtf[:, 2, :], cum3[:, 0, :], ALU.subtract, [b_dtf, b_cum3], [b_cres])
                CP("dve", cum3[:, 1, :], cres[:, 0, :], [b_cres], [b_cum3])
                TT("dve", cres[:, 1, :], cres[:, 0, :], cum3[:, 1, :], ALU.subtract, [b_cres, b_cum3], [b_cres])
                CP("dve", cum3[:, 2, :], cres[:, 1, :], [b_cres], [b_cum3])
            wb, bwb = load_win(l, G_DT, 8)
            pa, bpa = nextA()
            for kc in range(8):
                MM(pa[0:8, 0:BT], wb[:, kc, 0:8], hT[:, kc, :], kc == 0, kc == 7, [bwb, b_hT], [bpa])
            dt_chunk(0, pa, bpa)
            for t in range(TB):
                pt_, bpt_ = nextT()
                TR(pt_[:, 0:8], dtf[:, 0, t * 128:(t + 1) * 128], cst["ident"][0:8, 0:8], [b_dtf, cstb["ident"]], [bpt_])
                TR(pt_[:, 8:16], dtf[:, 2, t * 128:(t + 1) * 128], cst["ident"][0:8, 0:8], [b_dtf, cstb["ident"]], [bpt_])
                CP("dve", dttok[:, t, 0:16], pt_[:, 0:16], [bpt_], [b_dttok])

            if CUT <= 3:
                return
            def hirv(t, pt_, bpt_):
                pass
            def hirv_c(t, pt_, bpt_):
                CP("act", hvs[t][:], pt_[:, 0:256], [bpt_], [b_hvs[t]])
                CP("dve", rvbs[t][:], pt_[:, 256:512], [bpt_], [b_rvbs[t]])
            inproj_T(l, G_HIRV, hirv_c)
            if CUT == 35:
                return
            if full:
                def hqrq_chunk(ci, pa, bpa):
                    if ci < 2:
                        ACT(hrc[:, ci, :], hcum[:, ci, :], AF.Exp, [b_hcum, b_hrc], [b_hrc])
                        TT("dve", qp[:, ci, :], pa[:, 0:BT], hrc[:, ci, :], ALU.mult, [bpa, b_hrc], [b_qp])
                        ACT(hrc[:, ci, :], hcum[:, ci, :], AF.Exp, [b_hcum, b_hrc], [b_hrc], scale=-1.0)
                        TT("dve", kp[:, ci, :], hsg[:, ci, :], hrc[:, ci, :], ALU.mult, [b_hsg, b_hrc], [b_kp])
                        for t in range(TB):
                            CP("pool", qpm[:, ci, t, 32:64], qp[:, ci, t * 128 + 96:t * 128 + 128], [b_qp], [b_qpm])
                    else:
                        c = ci - 2
                        TT("dve", (rt1 if c == 0 else rt2)[:], pa[:, 0:BT], rtab[:, 0, :], ALU.mult, [bpa, b_rtab],
                           [b_rt1 if c == 0 else b_rt2])
                inproj_F(l, G_HQRQ, range(4), hqrq_chunk)

                def rqs_chunk(c, pa, bpa):
                    src, bsrc = (rt1, b_rt1) if c == 0 else (rt2, b_rt2)
                    STT(qr[:, c, :], pa[:, 0:BT], 1.0, rtab[:, 1, :], ALU.mult, ALU.mult, [bpa, b_rtab], [b_qr])
                    TT("dve", qr[:, c, :], qr[:, c, :], src[:], ALU.add, [b_qr, bsrc], [b_qr])
                    for t in range(TB):
                        TT("pool", qrg[:, c, t * 128:(t + 1) * 128], qr[:, c, t * 128:(t + 1) * 128], cst["gq"][:, c, :], ALU.mult,
                           [b_qr, cstb["gq"]], [b_qrg])
                inproj_F(l, G_RQS, range(2), rqs_chunk)
                def z_c(t, pt_, bpt_):
                    ACT(gzs[t][:, 0:512], pt_[:, 0:512], AF.Silu, [bpt_], [b_gzs[t]])
                inproj_T(l, G_Z, z_c)

                def hgrg_c(t, pt_, bpt_):
                    ACT(gzs[t][:, 512:768], pt_[:, 0:256], AF.Sigmoid, [bpt_], [b_gzs[t]])
                    ACT(gzs[t][:, 768:1024], pt_[:, 256:512], AF.Silu, [bpt_], [b_gzs[t]])
                inproj_T(l, G_HGRG, hgrg_c)

            if full:
                for g in range(2):
                    TS("dve", Cm[g][:], xbc[:, 5, :], gm[:, g:g + 1], None, ALU.mult, None, [b_xbc, b_gm], [b_Cm[g]])
            for h in range(8):
                g = h // 4
                pa, bpa = nextA()
                for q in range(3):
                    MM(pa[:, 0:BT], sel8all[:, h, :], cum3[:, q, :], q == 0, q == 2, [b_sel8all, b_cum3], [bpa])
                ACT(ecr[:], pa[:, 0:BT], AF.Exp, [bpa], [b_ecr])
                for t in range(TB):
                    CP("dve", dendb[:, t, h:h + 1], ecr[:, t * 128 + 127:t * 128 + 128], [b_ecr], [b_dendb])
                    CP("dve", dttok[:, t, 16 + h:17 + h], pa[:, t * 128 + 127:t * 128 + 128], [bpa], [b_dttok])

            if CUT <= 4 or CUT == 35:
                return
            for t in range(TB):
                i = t0 + t
                tsl = slice(t * 128, (t + 1) * 128)
                TT("dve", dttok[:, t, 24:32], dttok[:, t, 16:24], dttok[:, t, 8:16], ALU.subtract, [b_dttok], [b_dttok])
                ACT(dttok[:, t, 24:32], dttok[:, t, 24:32], AF.Exp, [b_dttok], [b_dttok])
                TT("dve", dttok[:, t, 32:40], dttok[:, t, 24:32], dttok[:, t, 0:8], ALU.mult, [b_dttok], [b_dttok])
                for c in range(4):
                    TR(pTRb[:, c * 128:(c + 1) * 128], xbc[:, c, tsl], identb[:], [b_xbc, b_identb], [b_pTR])
                TR(pTRb[:, 512:640], xbc[:, 4, tsl], identb[:], [b_xbc, b_identb], [b_pTR])
                xv = pTRb[:, 0:512].rearrange("p (h k) -> p h k", k=64)
                TT("dve", xs2[:].rearrange("p (h k) -> p h k", k=64), xv, dttok[:, t, 32:40].unsqueeze(2).broadcast_to([128, 8, 64]),
                   ALU.mult, [b_pTR, b_dttok], [b_xs2])
                if full:
                    TT("dve", xs1[:].rearrange("p (h k) -> p h k", k=64), xv, dttok[:, t, 0:8].unsqueeze(2).broadcast_to([128, 8, 64]),
                       ALU.mult, [b_pTR, b_dttok], [b_xs1])
                    TT("dve", xsd[:].rearrange("p (h k) -> p h k", k=64), xv, bcr[:, 768:776].unsqueeze(2).broadcast_to([128, 8, 64]),
                       ALU.mult, [b_pTR, b_bcr], [b_xsd])
                CP("act", Btok[:], pTRb[:, 512:640], [b_pTR], [b_Btok])
                hv, b_hv, rvb, b_rvb = hvs[t], b_hvs[t], rvbs[t], b_rvbs[t]
                for c in range(2):
                    TR(pTRb[:, c * 128:(c + 1) * 128], kend[:, c, tsl], identb[:], [b_kend, b_identb], [b_pTR])
                    TR(pTRb[:, 256 + c * 128:256 + (c + 1) * 128], kr[:, c, tsl], identb[:], [b_kr, b_identb], [b_pTR])
                CP("act", kendt[:], pTRb[:, 0:256], [b_pTR], [b_kendt])
                TS("dve", kendm[64:128, :], pTRb[64:128, 0:256], cst["mask96"][64:128, 0:1], None, ALU.mult, None,
                   [b_pTR, cstb["mask96"]], [b_kendm])
                TT("dve", krt[:].rearrange("p (h k) -> p h k", k=64), pTRb[:, 256:512].rearrange("p (h k) -> p h k", k=64),
                   cst["gk"][:].unsqueeze(2).broadcast_to([128, 4, 64]), ALU.mult, [b_pTR, cstb["gk"]], [b_krt])

                for sc in range(4):
                    sub = t * 4 + sc
                    for c in range(2):
                        if full:
                            CP("pool", Shs[c][sc][:], Sh[c][:], [b_Sh[c]], [b_Shs[c][sc]])
                        pa, bpa = nextA()
                        cs = slice(c * 128, (c + 1) * 128)
                        if sc < 3:
                            MM(pa[:, 0:128], kendt[sc * 32:(sc + 1) * 32, cs], hv[sc * 32:(sc + 1) * 32, cs], True, True, [b_kendt, b_hv], [bpa])
                        else:
                            MM(pa[:, 0:128], kendm[64:128, cs], hv[64:128, cs], True, True, [b_kendm, b_hv], [bpa])
                        STT(Sh[c][:], Sh[c][:], ecend[:, c, sub:sub + 1], pa[:, 0:128], ALU.mult, ALU.add, [b_Sh[c], b_ecend, bpa], [b_Sh[c]])

                if full and CUT2 >= 3:
                    for c in range(2):
                        TS("dve", qp0[:, c, :], qp[:, c, tsl], gm[:, 0:1], None, ALU.mult, None, [b_qp, b_gm], [b_qp0])
                        TS("dve", qr0[:, c, :], qr[:, c, tsl], gm[:, 0:1], None, ALU.mult, None, [b_qr, b_gm], [b_qr0])
                        TS("dve", qrg0[:, c, :], qrg[:, c, tsl], gm[:, 0:1], None, ALU.mult, None, [b_qrg, b_gm], [b_qrg0])
                        TS("dve", qpm0[:, c, :], qpm[:, c, t, :], gm[:, 0:1], None, ALU.mult, None, [b_qpm, b_gm], [b_qpm0])
                    for g in range(2):
                        MM(pSC[:, g * 128:(g + 1) * 128], xbc[:, 4, tsl], Cm[g][:, tsl], True, True, [b_xbc, b_Cm[g]], [b_pSC])
                    for h in range(8 if CUT2 != 313 else 0):
                        g = h // 4
                        k = sTrr[0] % 2; sTrr[0] += 1
                        pa, bpa = nextA()
                        for q in range(3):
                            MM(pa[:, 0:128], sel8all[:, h, :], cum3[:, q, tsl], q == 0, q == 2, [b_sel8all, b_cum3], [bpa])
                        STT(dm[:], pa[:, 0:128], dttok[:, t, 8 + h:9 + h], cst["negmask"][:], ALU.subtract, ALU.min,
                            [bpa, b_dttok, cstb["negmask"]], [b_dm])
                        ACT(ecr[:, 0:128], pa[:, 0:128], AF.Exp, [bpa], [b_ecr])
                        TT("dve", Cpj[k][:], Cm[g][:, tsl], ecr[:, 0:128], ALU.mult, [b_Cm[g], b_ecr], [b_Cpj[k]])
                        ACT(dm[:], dm[:], AF.Exp, [b_dm], [b_dm])
                        TT("dve", sT[k][:], pSC[:, g * 128:(g + 1) * 128], dm[:], ALU.mult, [b_pSC, b_dm], [b_sT[k]])
                        if CUT2 == 311:
                            continue
                        MM(pO[:, h * 64:(h + 1) * 64], sT[k][:], xs1[:, h * 64:(h + 1) * 64], True, CUT2 == 312, [b_sT[k], b_xs1], [b_pO])
                        if CUT2 == 312:
                            continue
                        MM(pO[:, h * 64:(h + 1) * 64], Cpj[k][:], Sssdb[:, h * 64:(h + 1) * 64], False, True, [b_Cpj[k], b_Sssdb], [b_pO])
                    for h in range(4 if CUT2 not in (31, 311, 312, 313) else 0):
                        c = h // 2
                        hr = slice((h % 2) * 64, (h % 2) * 64 + 64)
                        ev = (h % 2 == 0)
                        if ev:
                            MM(pSC[:, 256:384], kp[:, c, tsl], qp0[:, c, :], True, True, [b_kp, b_qp0], [b_pSC])
                        else:
                            MM(pSC[:, 256:384], kp[hr, c, tsl], qp[hr, c, tsl], True, True, [b_kp, b_qp], [b_pSC])
                        k = sTrr[0] % 2; sTrr[0] += 1
                        TT("dve", sT[k][:], pSC[:, 256:384], cst["blk32"][:], ALU.mult, [b_pSC, cstb["blk32"]], [b_sT[k]])
                        oc = slice(512 + h * 64, 512 + (h + 1) * 64)
                        for sc in (3, 2, 0, 1):
                            hcols = slice((h % 2) * 64, (h % 2) * 64 + 64)
                            if ev:
                                rhs = Shs[c][sc][:, hcols]
                                if sc < 3:
                                    MM(pO[sc * 32:(sc + 1) * 32, oc], qp0[:, c, sc * 32:(sc + 1) * 32], rhs, sc != 2, False,
                                       [b_qp0, b_Shs[c][sc]], [b_pO])
                                else:
                                    MM(pO[64:128, oc], qpm0[:, c, :], rhs, True, False, [b_qpm0, b_Shs[c][sc]], [b_pO])
                            else:
                                rhs = Shs[c][sc][hr, hcols]
                                if sc < 3:
                                    MM(pO[sc * 32:(sc + 1) * 32, oc], qp[hr, c, t * 128 + sc * 32:t * 128 + (sc + 1) * 32], rhs, sc != 2, False,
                                       [b_qp, b_Shs[c][sc]], [b_pO])
                                else:
                                    MM(pO[64:128, oc], qpm[hr, c, t, :], rhs, True, False, [b_qpm, b_Shs[c][sc]], [b_pO])
                        MM(pO[:, oc], sT[k][:], hv[:, h * 64:(h + 1) * 64], False, True, [b_sT[k], b_hv], [b_pO])
                    for h in range(4 if CUT2 not in (31, 32, 311, 312, 313) else 0):
                        c = h // 2
                        hr = slice((h % 2) * 64, (h % 2) * 64 + 64)
                        ev = (h % 2 == 0)
                        if ev:
                            MM(pSC[:, 384:512], kr[:, c, tsl], qr0[:, c, :], True, True, [b_kr, b_qr0], [b_pSC])
                        else:
                            MM(pSC[:, 384:512], kr[hr, c, tsl], qr[hr, c, tsl], True, True, [b_kr, b_qr], [b_pSC])
                        k = sTrr[0] % 2; sTrr[0] += 1
                        TT("dve", sT[k][:], pSC[:, 384:512], cst["retmask"][:, h, :], ALU.mult, [b_pSC, cstb["retmask"]], [b_sT[k]])
                        oc = slice(768 + h * 64, 768 + (h + 1) * 64)
                        MM(pO[:, oc], sT[k][:], rvb[:, h * 64:(h + 1) * 64], True, False, [b_sT[k], b_rvb], [b_pO])
                        if ev:
                            MM(pO[:, oc], qrg0[:, c, :], Srb[c][:, 0:64], False, True, [b_qrg0, b_Srb[c]], [b_pO])
                        else:
                            MM(pO[:, oc], qrg[hr, c, tsl], Srb[c][hr, 64:128], False, True, [b_qrg, b_Srb[c]], [b_pO])

                pa, bpa = nextA()
                MM(pa[:], Btok[:], xs2[:], True, True, [b_Btok, b_xs2], [bpa])
                TT("dve", Sssd[:].rearrange("p (h k) -> p h k", k=64), Sssd[:].rearrange("p (h k) -> p h k", k=64),
                   dendb[:, t, :].unsqueeze(2).broadcast_to([128, 8, 64]), ALU.mult, [b_Sssd, b_dendb], [b_Sssd])
                TT("dve", Sssd[:], Sssd[:], pa[:], ALU.add, [b_Sssd, bpa], [b_Sssd])
                if full:
                    CP("pool", Sssdb[:], Sssd[:], [b_Sssd], [b_Sssdb])
                if not full:
                    TT("dve", Dtot[:], Dtot[:], dendb[:, t, :], ALU.mult, [b_Dtot, b_dendb], [b_Dtot])
                for c in range(2):
                    pa, bpa = nextA()
                    cs = slice(c * 128, (c + 1) * 128)
                    MM(pa[:, 0:128], krt[:, cs], rvb[:, cs], True, True, [b_krt, b_rvb], [bpa])
                    STT(Sr[c][:], Sr[c][:], cst["g128"][:, c:c + 1], pa[:, 0:128], ALU.mult, ALU.add, [b_Sr[c], cstb["g128"], bpa], [b_Sr[c]])
                    if full:
                        CP("pool", Srb[c][:], Sr[c][:], [b_Sr[c]], [b_Srb[c]])

                if full and CUT2 >= 4 and CUT2 not in (31, 32, 311, 312, 313):
                    finish_tile(l, t, i)
            return


        def finish_tile(l, t, i):
            tsl = slice(t * 128, (t + 1) * 128)
            gz, b_gz = gzs[t], b_gzs[t]
            TT("dve", yy[:, 0:512], pO[:, 0:512], xsd[:], ALU.add, [b_pO, b_xsd], [b_yy])
            TT("dve", yy[:, 0:512], yy[:, 0:512], gz[:, 0:512], ALU.mult, [b_yy, b_gz], [b_yy])
            CP("act", yy[:, 512:1024], pO[:, 512:1024], [b_pO], [b_yy])
            TT("dve", otmp[:], yy[:, 0:512], yy[:, 0:512], ALU.mult, [b_yy], [b_otmp])
            RED(nst[:, 0:8], otmp[:].rearrange("p (g k) -> p g k", k=64), ALU.add, [b_otmp], [b_nst])
            TT("dve", otmp[:], yy[:, 512:1024], yy[:, 512:1024], ALU.mult, [b_yy, b_otmp], [b_otmp])
            RED(nst[:, 8:16], otmp[:].rearrange("p (g k) -> p g k", k=64), ALU.add, [b_otmp], [b_nst])
            RED(nst[:, 16:18], nst[:, 0:8].rearrange("p (g k) -> p g k", k=4), ALU.add, [b_nst], [b_nst])
            TS("dve", nst[:, 16:18], nst[:, 16:18], 1.0 / 256, EPS, ALU.mult, ALU.add, [b_nst], [b_nst])
            TS("dve", nst[:, 8:16], nst[:, 8:16], 1.0 / 64, EPS, ALU.mult, ALU.add, [b_nst], [b_nst])
            ACT(nst[:, 8:18], nst[:, 8:18], AF.Sqrt, [b_nst], [b_nst])
            RECIP(nst[:, 8:18], nst[:, 8:18], [b_nst], [b_nst])
            for g in range(2):
                STT(mixed[:, g * 256:(g + 1) * 256], yy[:, g * 256:(g + 1) * 256], nst[:, 16 + g:17 + g], bcr[:, g * 256:(g + 1) * 256],
                    ALU.mult, ALU.mult, [b_yy, b_nst, b_bcr], [b_mixed])
            TT("dve", yy[:, 512:1024].rearrange("p (h k) -> p h k", k=64), yy[:, 512:1024].rearrange("p (h k) -> p h k", k=64),
               nst[:, 8:16].unsqueeze(2).broadcast_to([128, 8, 64]), ALU.mult, [b_yy, b_nst], [b_yy])
            TT("dve", yy[:, 512:768], yy[:, 512:768], bcr[:, 512:768], ALU.mult, [b_yy, b_bcr], [b_yy])
            TT("dve", mixed[:, 512:1024], yy[:, 512:1024], gz[:, 512:1024], ALU.mult, [b_yy, b_gz], [b_mixed])
            for j in range(8):
                TR(pTRb[:, j * 128:(j + 1) * 128], mixed[:, j * 128:(j + 1) * 128], identb[:], [b_mixed, b_identb], [b_pTR])
            CP("act", mT[:], pTRb[:].rearrange("p (a b) -> p a b", b=128), [b_pTR], [b_mT])
            for half in range(2):
                pa, bpa = nextA()
                for kc in range(8):
                    MM(pa[:], mT[:, kc, :], wout[:, kc, half * 512:(half + 1) * 512], kc == 0, kc == 7, [b_mT, b_wgu[0], b_wgu[1]], [bpa])
                TT("dve", otmp[:], pa[:], gbc[:, 0, half * 512:(half + 1) * 512], ALU.mult, [bpa, b_gbc[0]], [b_otmp])
                TT("dve", X[:, i, half * 512:(half + 1) * 512], X[:, i, half * 512:(half + 1) * 512], otmp[:], ALU.add,
                   [bX[i], b_otmp], [bX[i]])

        def barrier():
            allb = carved + list(b_hTf) + [b_yy, b_cbs, b_sgs, b_gzs[0], b_gzs[1]] + list(b_hids)
            S.op("dve", lambda e: e.memset(ccdummy[:, 1:2], 0.0), [], allb + [b_ccd])

        def reset_states(one_for_dtot=True):
            if stage != 2.7:
                MS("dve", Sssd[:], 0.0, [b_Sssd])
            for c in range(2):
                if stage != 2.7:
                    MS("dve", Sh[c][:], 0.0, [b_Sh[c]])
                    MS("dve", Sr[c][:], 0.0, [b_Sr[c]])
            if stage != 2.7:
                MS("dve", Dtot[:], 1.0, [b_Dtot])
                MS("dve", Lsum[:], 0.0, [b_Lsum])
            MS("pool", qpm[:], 0.0, [b_qpm])
            for h in range(8):
                CP("dve", sel8all[:, h, :], identb[0:8, h:h + 1].broadcast_to([8, 128]), [b_identb], [b_sel8all])

        def halo_exchange(l):
            for t in range(TB):
                norm_to_hT(NT - TB + t, hT, b_hT, t * 128, 0, 0)

            MS("dve", snd[:, 0:256], 0.0, [b_snd])

            def grab(base):
                def f(ci, pa, bpa):
                    c = base + ci
                    CP("dve", snd[:, c * 3:(c + 1) * 3], pa[:, BT - 3:BT], [bpa], [b_snd])
                return f
            inproj_F(l, G_XS, range(4), grab(0))
            inproj_F(l, G_BCF, range(2), grab(4))
            if stage == 2:
                return
            gath, b_g = allgather(snd[:, 0:256], 256, [b_snd], None)
            if stage == 2.2:
                return
            MS("dve", halo[:], 0.0, [b_halo])
            for r in range(4):
                DMA("sp", rcv[:, 0:18], gath[r * 128:(r + 1) * 128, 0:18], [b_g], [b_rcv])
                STT(halo[:], rcv[:, 0:18], prevs[:, r:r + 1], halo[:], ALU.mult, ALU.add, [b_rcv, b_prevs, b_halo], [b_halo])

        def state_exchange():
            CP("dve", snd[:, 0:512], Sssd[:], [b_Sssd], [b_snd])
            for c in range(2):
                CP("dve", snd[:, 512 + c * 128:640 + c * 128], Sh[c][:], [b_Sh[c]], [b_snd])
                CP("dve", snd[:, 768 + c * 128:896 + c * 128], Sr[c][:], [b_Sr[c]], [b_snd])
            CP("dve", snd[:, 1024:1032], Dtot[:], [b_Dtot], [b_snd])
            ACT(snd[:, 1032:1034], Lsum[:, 0:2], AF.Exp, [b_Lsum], [b_snd])
            MS("dve", snd[:, 1034:NS], 0.0, [b_snd])
            gath, b_g = allgather(snd[:, 0:NS], NS, [b_snd], None)
            MS("dve", Sssd[:], 0.0, [b_Sssd])
            for c in range(2):
                MS("dve", Sh[c][:], 0.0, [b_Sh[c]])
                MS("dve", Sr[c][:], 0.0, [b_Sr[c]])
            for r in range(4):
                DMA("sp", rcv[:, 0:NS], gath[r * 128:(r + 1) * 128, :], [b_g], [b_rcv])
                m = segm[:, r:r + 1]
                v3 = lambda a: a.rearrange("p (h k) -> p h k", k=64)
                TT("dve", v3(xt1[:]), v3(Sssd[:]), rcv[:, 1024:1032].unsqueeze(2).broadcast_to([128, 8, 64]), ALU.mult,
                   [b_Sssd, b_rcv], [b_xt1])
                TT("dve", xt1[:], xt1[:], rcv[:, 0:512], ALU.add, [b_xt1, b_rcv], [b_xt1])
                TT("dve", xt1[:], xt1[:], Sssd[:], ALU.subtract, [b_xt1, b_Sssd], [b_xt1])
                STT(Sssd[:], xt1[:], m, Sssd[:], ALU.mult, ALU.add, [b_xt1, b_segm, b_Sssd], [b_Sssd])
                for c in range(2):
                    for (St, bSt, off, sc_ap, sc_b) in ((Sh[c], b_Sh[c], 512 + c * 128, rcv[:, 1032 + c:1033 + c], b_rcv),
                                                       (Sr[c], b_Sr[c], 768 + c * 128, cst["g2048"][:, c:c + 1], cstb["g2048"])):
                        STT(xt1[:, 0:128], St[:], sc_ap, rcv[:, off:off + 128], ALU.mult, ALU.add, [bSt, sc_b, b_rcv], [b_xt1])
                        TT("dve", xt1[:, 0:128], xt1[:, 0:128], St[:], ALU.subtract, [b_xt1, bSt], [b_xt1])
                        STT(St[:], xt1[:, 0:128], m, St[:], ALU.mult, ALU.add, [b_xt1, b_segm, bSt], [bSt])
            CP("pool", Sssdb[:], Sssd[:], [b_Sssd], [b_Sssdb])
            for c in range(2):
                CP("pool", Srb[c][:], Sr[c][:], [b_Sr[c]], [b_Srb[c]])

        def moe(l):
            DMA("pool", wr[:], wr_d[l].rearrange("(kc p) c -> p kc c", p=128), [], [b_wr])
            for i in range(NT):
                norm_to_hT(i, hTf, b_hTf[i // 4], i * 128, 8, 16)
                pa, bpa = nextA()
                for kc in range(8):
                    MM(pa[:, 0:36], hTf[:, kc, i * 128:(i + 1) * 128], wr[:, kc, :], kc == 0, kc == 7, [b_hTf[i // 4], b_wr], [bpa])
                TT("dve", lg_sb[:], pa[:, 0:36], bcr[:, 776:812], ALU.add, [bpa, b_bcr], [b_lg])
                R_, W_ = [b_lg, b_rt], [b_rt]
                RED(rt[:, 0:1], lg_sb[:, 0:4], ALU.max, R_, W_)
                TS("dve", rt[:, 4:8], lg_sb[:, 0:4], rt[:, 0:1], None, ALU.is_equal, None, R_, W_)
                TS("dve", rt[:, 1:2], rt[:, 0:1], -1.0, None, ALU.mult, None, R_, W_)
                ACT(rt[:, 8:12], lg_sb[:, 0:4], AF.Exp, R_, W_, bias=rt[:, 1:2], accum=rt[:, 2:3])
                RECIP(rt[:, 3:4], rt[:, 2:3], R_, W_)
                TT("dve", rt[:, 16:48].rearrange("p (g e) -> p g e", e=8), lg_sb[:, 4:36].rearrange("p (g e) -> p g e", e=8),
                   rt[:, 4:8].unsqueeze(2).broadcast_to([128, 4, 8]), ALU.mult, R_, W_)
                RED(rt[:, 48:56], rt[:, 16:48].rearrange("p (g e) -> p e g", e=8), ALU.add, R_, W_)
                RED(rt[:, 56:57], rt[:, 48:56], ALU.max, R_, W_)
                TS("dve", rt[:, 16:24], rt[:, 48:56], rt[:, 56:57], None, ALU.is_equal, None, R_, W_)
                STT(rt[:, 24:32], rt[:, 16:24], NEG, rt[:, 48:56], ALU.mult, ALU.add, R_, W_)
                RED(rt[:, 57:58], rt[:, 24:32], ALU.max, R_, W_)
                TS("dve", rt[:, 32:40], rt[:, 24:32], rt[:, 57:58], None, ALU.is_equal, None, R_, W_)
                TT("dve", rt[:, 58:59], rt[:, 56:57], rt[:, 57:58], ALU.subtract, R_, W_)
                ACT(rt[:, 59:60], rt[:, 58:59], AF.Sigmoid, R_, W_)
                TT("dve", rt[:, 60:61], rt[:, 59:60], rt[:, 3:4], ALU.mult, R_, W_)
                TT("dve", rt[:, 61:62], rt[:, 3:4], rt[:, 60:61], ALU.subtract, R_, W_)
                TS("dve", rt[:, 40:48], rt[:, 16:24], rt[:, 60:61], None, ALU.mult, None, R_, W_)
                STT(rt[:, 40:48], rt[:, 32:40], rt[:, 61:62], rt[:, 40:48], ALU.mult, ALU.add, R_, W_)
                TT("dve", comb[:].rearrange("p (g e) -> p g e", e=8), rt[:, 4:8].unsqueeze(2).broadcast_to([128, 4, 8]),
                   rt[:, 40:48].unsqueeze(1).broadcast_to([128, 4, 8]), ALU.mult, [b_rt], [b_comb])
                pt_, bpt_ = nextT()
                TR(pt_[0:32, 0:128], comb[:], cst["ident"][:], [b_comb, cstb["ident"]], [bpt_])
                CP("act", combT[:, i * 128:(i + 1) * 128], pt_[0:32, 0:128], [bpt_], [b_combT[i // 4]])
            pdr = [0]

            def load_expert(e):
                k = e % 2
                DMA("pool", wgu[k][:, :, 0:256], wg_d[l, e].rearrange("(kc p) c -> p kc c", p=128), [], [b_wgu[k]])
                DMA("pool", wgu[k][:, :, 256:512], wu_d[l, e].rearrange("(kc p) c -> p kc c", p=128), [], [b_wgu[k]])
                DMA("pool", wdn[k][:], wd_d[l, e].rearrange("(kc p) c -> p kc c", p=128), [], [b_wdn[k]])
                TT("pool", wdn[k][:], wdn[k][:], gbc[:, 1, :].unsqueeze(1).broadcast_to([128, 2, D]), ALU.mult, [b_wdn[k], b_gbc[1]], [b_wdn[k]])

            def gateup(e, tb):
                k = e % 2
                tok = slice(tb * 512, (tb + 1) * 512)
                CP("pool", sel32[:], identb[0:32, e:e + 1].broadcast_to([32, 128]), [b_identb], [b_sel32])
                pa, bpa = nextA()
                MM(pa[:], sel32[:], combT[:, tok], True, True, [b_sel32, b_combT[tb]], [bpa])
                CP("act", cbs[:], pa[:], [bpa], [b_cbs])
                hi_ = hidrr[0] % 2
                hidrr[0] += 1
                hid, b_hid = hids[hi_], b_hids[hi_]
                for hc in range(2):
                    pg, bpg = nextA()
                    for kc in range(8):
                        MM(pg[:], wgu[k][:, kc, hc * 128:(hc + 1) * 128], hTf[:, kc, tok], kc == 0, kc == 7, [b_wgu[k], b_hTf[tb]], [bpg])
                    pu, bpu = nextT()
                    for kc in range(8):
                        MM(pu[:], wgu[k][:, kc, 256 + hc * 128:256 + (hc + 1) * 128], hTf[:, kc, tok], kc == 0, kc == 7, [b_wgu[k], b_hTf[tb]], [bpu])
                    ACT(sgs[:], pg[:], AF.Silu, [bpg], [b_sgs])
                    TT("dve", tmo[:], sgs[:], pu[:], ALU.mult, [b_sgs, bpu], [b_tmo])
                    TT("pool", hid[:, hc, :], tmo[:], cbs[:], ALU.mult, [b_tmo, b_cbs], [b_hid])
                return hi_

            def down(e, tb, hi_):
                k = e % 2
                hid, b_hid = hids[hi_], b_hids[hi_]
                for tt in range(4):
                    i = tb * 4 + tt
                    for half in range(2):
                        if pdr[0] % 2 == 0:
                            pd, bpd = pSC[:, :], b_pSC
                        else:
                            pd, bpd = pO[:, 0:512], b_pO
                        pdr[0] += 1
                        for hc in range(2):
                            MM(pd, hid[:, hc, tt * 128:(tt + 1) * 128], wdn[k][:, hc, half * 512:(half + 1) * 512], hc == 0, hc == 1,
                               [b_hid, b_wdn[k]], [bpd])
                        xs_ = X[:, i, half * 512:(half + 1) * 512]
                        TT("dve", xs_, xs_, pd, ALU.add, [bX[i], bpd], [bX[i]])

            pending = None
            load_expert(0)
            for e in range(32):
                for tb in range(4):
                    hi_ = gateup(e, tb)
                    if pending is not None:
                        down(*pending)
                    pending = (e, tb, hi_)
                    if tb == 0 and e + 1 < 32:
                        load_expert(e + 1)
            down(*pending)

        convert_win(0)
        for l in range(n_layers):
            if stage < 1:
                break
            layer_params(l)
            barrier()
            if stage < 2:
                break
            if stage == 2.5:
                reset_states()
                break
            if stage not in (2, 2.2, 2.4):
                reset_states()
            halo_exchange(l)
            if stage in (2, 2.2, 2.4):
                break
            if stage < 3:
                break
            for blk in range(NBLK if CUT > 10 else 1):
                mixer_block(l, blk, False)
            if stage < 4:
                break
            state_exchange()
            DMA("pool", wout[:, :, :], wout_d[l].rearrange("(kc p) c -> p kc c", p=128), [], [b_wgu[0], b_wgu[1]])
            if stage < 5:
                break
            for blk in range(NBLK):
                mixer_block(l, blk, True)
            barrier()
            if stage < 6:
                break
            if l + 1 < n_layers:
                convert_win(l + 1)
            moe(l)
        finals = []
        barrier()
        if do_final:
            DMA("sp", gbc[:, 0, :], fng_d.broadcast_to([128, D]), [], [b_gbc[0]])
            for i in range(NT):
                ACT(yy[:, 0:D], X[:, i, :], AF.Square, [bX[i]], [b_yy, b_ssq], accum=ssq[:, 0:1])
                rstd_from_ssq(0, 1, 1.0 / D)
                STT(yy[:, 0:D], X[:, i, :], ssq[:, 0:1], gbc[:, 0, :], ALU.mult, ALU.mult, [bX[i], b_ssq, b_gbc[0]], [b_yy])
                finals.append(DMA("sp", out_d[i * 128:(i + 1) * 128, :], yy[:, 0:D], [b_yy], [S.buf()]))
        else:
            for i in range(NT):
                finals.append(DMA("sp", out_d[i * 128:(i + 1) * 128, :], X[:, i, :], [bX[i]], [S.buf()]))
        S.emit(final_waits=finals)
    return nc


def _prep_inputs(inp):
    f = lambda a: np.ascontiguousarray(np.asarray(a, dtype=np.float32))
    w_in = f(inp["w_in"])
    o = {}
    off = 0
    for name, w in (("z", 512), ("xs", 512), ("B", 128), ("C", 128), ("dt", 8), ("hq", 256), ("hf", 256), ("hi", 256),
                    ("hg", 256), ("rq", 256), ("rk", 256), ("rv", 256), ("rg", 256)):
        o[name] = (off, off + w)
        off += w
    sl = lambda n: w_in[:, :, o[n][0]:o[n][1]]

    def swap(a):
        a4 = a.reshape(a.shape[0], a.shape[1], 4, 2, 32)
        return np.ascontiguousarray(a4[:, :, :, ::-1, :]).reshape(a.shape)
    w_in_p = np.concatenate([sl("xs"), sl("B"), sl("C"), sl("hf"), sl("rk"), swap(sl("rk")), sl("hi"), sl("rv"), sl("dt"),
                             sl("hq"), sl("rq"), swap(sl("rq")), sl("z"), sl("hg"), sl("rg")], axis=2)
    assert w_in_p.shape[2] == DINP
    conv_w = f(inp["conv_w"]); conv_b = f(inp["conv_b"]); lbr = f(inp["hgrn_lower_bounds"])
    pm = np.zeros((DEPTH, NPM, 128), np.float32)
    for l in range(DEPTH):
        pm[l, 0:8] = f(inp["norm_mix_g"])[l].reshape(8, 128)
        pm[l, 8:16] = f(inp["norm_ffn_g"])[l].reshape(8, 128)
        for c in range(6):
            for j in range(4):
                pm[l, 16 + c * 4 + j] = conv_w[l, j, c * 128:(c + 1) * 128]
            pm[l, 40 + c] = conv_b[l, c * 128:(c + 1) * 128]
        for lp in range(DEPTH):
            for c in range(2):
                pm[l, 46 + 2 * lp + c] = lbr[lp, c * 128:(c + 1) * 128]
    bcrow = np.concatenate([f(inp["ssd_norm_g"]), f(inp["hgrn_norm_g"]), f(inp["ssd_d"]), f(inp["b_grp"]), f(inp["b_exp"])], axis=1)
    dtp = np.stack([f(inp["ssd_dt_bias"]), f(inp["ssd_a_log"])], axis=2)
    w_r = np.concatenate([f(inp["w_grp"]), f(inp["w_exp"])], axis=2)
    shared = {
        "pm": pm, "bcrow": np.ascontiguousarray(bcrow), "dtp": np.ascontiguousarray(dtp),
        "final_g": f(inp["final_norm_g"]).reshape(1, D), "w_ada": f(inp["w_ada"]), "b_ada": f(inp["b_ada"]),
        "w_in": np.ascontiguousarray(w_in_p), "w_out": f(inp["w_out"]), "w_r": np.ascontiguousarray(w_r),
        "w_gate": f(inp["w_gate"]), "w_up": f(inp["w_up"]), "w_down": f(inp["w_down"]),
    }
    x = f(inp["x"]); c = f(inp["c"])
    maps = []
    for core in range(NCORE):
        b, seg = core // 4, core % 4
        m = dict(shared)
        m["x"] = np.ascontiguousarray(x[b, seg * SEG:(seg + 1) * SEG, :])
        m["cT"] = np.ascontiguousarray(c[b].reshape(8, 128).T)
        sm = np.zeros((128, 4), np.float32); sm[:, :seg] = 1.0
        pv = np.zeros((128, 4), np.float32)
        if seg > 0:
            pv[:, seg - 1] = 1.0
        m["segmask"] = sm
        m["prevsel"] = pv
        for k, v in _consts(seg).items():
            m["c_" + k] = np.ascontiguousarray(v.astype(np.float32))
        maps.append(m)
    return maps


def kernel(**inputs):
    maps = _prep_inputs(inputs)
    nc = build()
    res = run_bass_kernel_spmd(nc, maps, core_ids=list(range(NCORE)))
    out = np.zeros((2, 4 * SEG, D), np.float32)
    for core in range(NCORE):
        b, seg = core // 4, core % 4
        out[b, seg * SEG:(seg + 1) * SEG, :] = np.asarray(res.results[core]["out"], dtype=np.float32)
    return out
```

```python
import contextlib
import math
import numpy as np
import concourse.bass as bass
import concourse.mybir as mybir
from concourse.bass_utils import run_bass_kernel_spmd

F32 = mybir.dt.float32
BF16 = mybir.dt.bfloat16
AF = mybir.ActivationFunctionType
ALU = mybir.AluOpType
AX = mybir.AxisListType

EPOCH = 4000
D = 1024
NT = 16
TB = 2
BT = 128 * TB
NBLK = NT // TB
NCORE = 8
SEG = 2048
DEPTH = 4
EPS = 1e-6
NEG = -1.0e30
CUT = 99
CUT2 = 99

G_XS, G_BCF, G_RK, G_HIRV, G_DT, G_HQRQ, G_RQS, G_Z, G_HGRG = range(9)
GCOLS = [512, 512, 512, 512, 8, 512, 256, 512, 512]
GOFF = [0]
for _c in GCOLS:
    GOFF.append(GOFF[-1] + _c)
DINP = GOFF[-1]


class Buf:
    __slots__ = ("name", "w", "r", "excl")

    def __init__(self, name):
        self.name = name
        self.w = None
        self.r = []
        self.excl = False


class Op:
    __slots__ = ("eng", "idx", "fn", "deps", "dma", "done", "inc")

    def __init__(self, eng, idx, fn, deps, dma):
        self.eng = eng
        self.idx = idx
        self.fn = fn
        self.deps = deps
        self.dma = dma
        self.done = None
        self.inc = False


class Sched:
    ENGS = ("pe", "act", "dve", "pool", "sp")

    def __init__(self, nc, n_dma_sems=40, same_eng_sync=True):
        self.nc = nc
        self.ops = {e: [] for e in self.ENGS}
        self.same = same_eng_sync
        self.n_dma = n_dma_sems
        self.dma_uses = [0] * n_dma_sems
        self.dma_last = [None] * n_dma_sems
        n_sw = n_dma_sems // 2
        self.dma_ring = {"pool": (0, n_sw), "sp": (n_sw, n_dma_sems - n_sw), "act": (n_sw, n_dma_sems - n_sw)}
        self.dma_rr = {"pool": 0, "sp": 0, "act": 0}
        self.nbuf = 0

    def buf(self, name=None):
        self.nbuf += 1
        return Buf(name or f"b{self.nbuf}")

    def bufs(self, n, name="b"):
        return [self.buf(f"{name}{i}") for i in range(n)]

    def _deps(self, reads, writes):
        deps = []
        for b in reads:
            if b.w is not None:
                deps.append(b.w)
        for b in writes:
            if b.w is not None:
                deps.append(b.w)
            deps.extend(b.r)
        return deps

    def _commit(self, op, reads, writes):
        for b in reads:
            b.r.append(op)
        for b in writes:
            b.w = op
            b.r = []

    def op(self, eng, fn, reads=(), writes=()):
        if any(b.excl for b in reads):
            writes = list(writes) + [b for b in reads if b.excl and b not in writes]
            reads = [b for b in reads if not b.excl]
        deps = self._deps(reads, writes)
        o = Op(eng, len(self.ops[eng]), fn, deps, None)
        self.ops[eng].append(o)
        self._commit(o, reads, writes)
        return o

    def dma(self, eng, fn, reads=(), writes=()):
        deps = self._deps(reads, writes)
        base, n = self.dma_ring[eng]
        key = "sp" if eng == "act" else eng
        r = base + self.dma_rr[key]
        self.dma_rr[key] = (self.dma_rr[key] + 1) % n
        if self.dma_last[r] is not None:
            deps.append(self.dma_last[r])
        self.dma_uses[r] += 1
        o = Op(eng, len(self.ops[eng]), fn, deps, (r, 16 * self.dma_uses[r]))
        self.dma_last[r] = o
        self.ops[eng].append(o)
        self._commit(o, reads, writes)
        return o

    def _skip(self, d, e):
        return d.eng == e and (e == "pe" or e == "sp" or not self.same)

    def emit(self, final_waits=()):
        nc = self.nc
        for e in self.ENGS:
            for o in self.ops[e]:
                for d in o.deps:
                    if d.dma is None and not self._skip(d, e):
                        d.inc = True
        for o in final_waits:
            if o.dma is None:
                o.inc = True
        nsem = {}
        for e in self.ENGS:
            c = 0
            for o in self.ops[e]:
                if o.dma is None and o.inc:
                    o.done = (e, c // EPOCH, c % EPOCH + 1)
                    c += 1
            nsem[e] = (c + EPOCH - 1) // EPOCH
        with contextlib.ExitStack() as st:
            esems = {e: [st.enter_context(nc.semaphore(f"s_{e}{i}")) for i in range(nsem[e])]
                     for e in self.ENGS}
            dsems = [st.enter_context(nc.semaphore(f"s_dma{i}")) for i in range(self.n_dma)]
            block = st.enter_context(nc.Block())

            def run(e, eng):
                seen = {}
                for o in self.ops[e]:
                    for d in o.deps:
                        if d.dma is not None:
                            key = ("d", d.dma[0]); sem = dsems[d.dma[0]]; val = d.dma[1]
                        else:
                            if self._skip(d, e):
                                continue
                            key = (d.done[0], d.done[1]); sem = esems[d.done[0]][d.done[1]]; val = d.done[2]
                        if seen.get(key, 0) >= val:
                            continue
                        seen[key] = val
                        eng.wait_ge(sem, val)
                    ins = o.fn(eng)
                    if o.dma is not None:
                        ins.then_inc(dsems[o.dma[0]], 16)
                    elif o.inc:
                        ins.then_inc(esems[e][o.done[1]], 1)
                if e == "sp":
                    for o in final_waits:
                        if o.dma is not None:
                            eng.wait_ge(dsems[o.dma[0]], o.dma[1])
                        else:
                            eng.wait_ge(esems[o.done[0]][o.done[1]], o.done[2])

            @block.tensor
            def _(eng):
                run("pe", eng)

            @block.scalar
            def _(eng):
                run("act", eng)

            @block.vector
            def _(eng):
                run("dve", eng)

            @block.gpsimd
            def _(eng):
                run("pool", eng)

            @block.sync
            def _(eng):
                run("sp", eng)


def _consts(seg):
    c = {}
    p = np.arange(128)
    t = np.arange(128)
    c["ident"] = np.eye(128, dtype=np.float32)
    c["negmask"] = np.where(p[:, None] <= t[None, :], 0.0, NEG).astype(np.float32)
    c["blk32"] = ((p[:, None] <= t[None, :]) & (p[:, None] // 32 == t[None, :] // 32)).astype(np.float32)
    gam = 1.0 - 2.0 ** (-5.0 - np.arange(4))
    lg = np.log1p(-(2.0 ** (-5.0 - np.arange(4, dtype=np.float64))))
    dm = np.zeros((128, 4, 128), np.float32)
    for h in range(4):
        dm[:, h, :] = np.where(p[:, None] <= t[None, :], 0.125 * np.exp(lg[h] * (t[None, :] - p[:, None])), 0.0)
    c["retmask"] = dm
    gq = np.zeros((128, 2, 128), np.float32)
    g128 = np.zeros((128, 2), np.float32)
    g2048 = np.zeros((128, 2), np.float32)
    for cp in range(2):
        for hh in range(2):
            h = cp * 2 + hh
            gq[hh * 64:(hh + 1) * 64, cp, :] = np.exp(lg[h] * (t + 1))[None, :]
            g128[hh * 64:(hh + 1) * 64, cp] = np.exp(lg[h] * 128)
            g2048[hh * 64:(hh + 1) * 64, cp] = np.exp(lg[h] * SEG)
    c["gq"] = gq
    c["g128"] = g128
    c["g2048"] = g2048
    gk = np.zeros((128, 4), np.float32)
    for h in range(4):
        gk[:, h] = 0.125 * np.exp(lg[h] * (127 - p))
    c["gk"] = gk
    r128 = np.ones((128, BT), np.float32); r128[:, ::128] = 0.0
    r32 = np.ones((128, BT), np.float32); r32[:, ::32] = 0.0
    c["reset128"] = r128
    c["reset32"] = r32
    c["mask96"] = (p >= 96).astype(np.float32)[:, None]
    f = p % 64
    invf = (10000.0 ** (-(f % 32).astype(np.float32) / np.float32(32))).astype(np.float32)
    pos = (seg * SEG + np.arange(SEG)).astype(np.float32)
    ang = (pos[None, :] * invf[:, None]).astype(np.float32)
    sgn = np.where(f < 32, -1.0, 1.0).astype(np.float32)
    c["rcos"] = np.cos(ang).astype(np.float32)
    c["rsin"] = (np.sin(ang) * sgn[:, None]).astype(np.float32)
    return c


CONST_SHAPES = {
    "ident": [128, 128], "negmask": [128, 128], "blk32": [128, 128], "retmask": [128, 4, 128],
    "gq": [128, 2, 128], "g128": [128, 2], "g2048": [128, 2], "gk": [128, 4],
    "reset128": [128, BT], "reset32": [128, BT],
    "mask96": [128, 1], "rcos": [128, SEG], "rsin": [128, SEG],
}

NPM = 54
NBC = 812


def build(n_layers=DEPTH, do_final=True, dbg=False, stage=9, fake_cc=False):
    nc = bass.Bass("TRN2", target_bir_lowering=False)

    def din(name, shape):
        return nc.dram_tensor(name, shape, F32, kind="ExternalInput").ap()

    x_d = din("x", [SEG, D])
    cT_d = din("cT", [128, 8])
    pm_d = din("pm", [DEPTH, NPM, 128])
    bc_d = din("bcrow", [DEPTH, NBC])
    dtp_d = din("dtp", [DEPTH, 8, 2])
    fng_d = din("final_g", [1, D])
    wada_d = din("w_ada", [DEPTH, D, 6 * D])
    bada_d = din("b_ada", [DEPTH, 6 * D])
    win_d = din("w_in", [DEPTH, D, DINP])
    wout_d = din("w_out", [DEPTH, D, D])
    wr_d = din("w_r", [DEPTH, D, 36])
    _ed = (DEPTH if n_layers > 1 else 1, 32) if stage >= 6 else (1, 1)
    wg_d = din("w_gate", [_ed[0], _ed[1], D, 256])
    wu_d = din("w_up", [_ed[0], _ed[1], D, 256])
    wd_d = din("w_down", [_ed[0], _ed[1], 256, D])
    segm_d = din("segmask", [128, 4])
    prev_d = din("prevsel", [128, 4])
    cst_d = {k: din("c_" + k, v) for k, v in CONST_SHAPES.items()}
    out_d = nc.dram_tensor("out", [SEG, D], F32, kind="ExternalOutput").ap()
    NS = 1088

    win_bf = [nc.dram_tensor(f"win_bf{l}", [D, DINP], BF16) for l in range(DEPTH)]
    S = Sched(nc)
    b_winbf = S.bufs(DEPTH, "winbf")
    st = contextlib.ExitStack()
    with st:
        def sb(name, shape, dt=F32):
            return st.enter_context(nc.sbuf_tensor("s_" + name, shape, dt))

        def ps(name, shape, dt=F32):
            return st.enter_context(nc.psum_tensor("p_" + name, shape, dt))

        ccsem = st.enter_context(nc.semaphore("ccsem"))
        cc_count = [0]

        def MM(out, lhsT, rhs, start, stop, R, W):
            return S.op("pe", lambda e: e.matmul(out, lhsT=lhsT, rhs=rhs, start=start, stop=stop), R, W)

        def TR(out, in_, idn, R, W):
            return S.op("pe", lambda e: e.transpose(out, in_, idn), R, W)

        def ACT(out, in_, func, R, W, bias=None, scale=None, accum=None):
            kw = {}
            if bias is not None:
                kw["bias"] = bias
            if scale is not None:
                kw["scale"] = scale
            if accum is not None:
                kw["accum_out"] = accum
            return S.op("act", lambda e: e.activation(out=out, in_=in_, func=func, **kw), R, W)

        def TS(eng, out, in0, s1, s2, op0, op1, R, W):
            if op1 is None:
                return S.op(eng, lambda e: e.tensor_scalar(out=out, in0=in0, scalar1=s1, scalar2=None, op0=op0), R, W)
            return S.op(eng, lambda e: e.tensor_scalar(out=out, in0=in0, scalar1=s1, scalar2=s2, op0=op0, op1=op1), R, W)

        def TT(eng, out, in0, in1, op, R, W):
            return S.op(eng, lambda e: e.tensor_tensor(out=out, in0=in0, in1=in1, op=op), R, W)

        def STT(out, in0, scalar, in1, op0, op1, R, W):
            return S.op("dve", lambda e: e.scalar_tensor_tensor(out=out, in0=in0, scalar=scalar, in1=in1, op0=op0, op1=op1), R, W)

        def CP(eng, out, in_, R, W):
            if eng == "act":
                return S.op(eng, lambda e: e.activation(out=out, in_=in_, func=AF.Copy), R, W)
            return S.op(eng, lambda e: e.tensor_copy(out=out, in_=in_), R, W)

        def MS(eng, out, val, W):
            return S.op(eng, lambda e: e.memset(out, val), (), W)

        def DMA(q, out, in_, R, W):
            return S.dma(q, lambda e: e.dma_start(out=out, in_=in_), R, W)

        def RED(out, in_, op, R, W):
            return S.op("dve", lambda e: e.tensor_reduce(out=out, in_=in_, axis=AX.X, op=op), R, W)

        def RECIP(out, in_, R, W):
            return S.op("dve", lambda e: e.reciprocal(out=out, in_=in_), R, W)

        def SCAN(out, d0, d1, R, W):
            return S.op("dve", lambda e: e.tensor_tensor_scan(out=out, data0=d0, data1=d1, initial=0.0,
                                                              op0=ALU.mult, op1=ALU.add), R, W)

        def allgather(src_sb, width, R, W_rcv):
            i = cc_count[0]
            cc_count[0] += 1
            b_in, b_out = S.buf(), S.buf()
            cin_t = nc.dram_tensor(f"ccin_{i}", [128, width], F32)
            cout_t = nc.dram_tensor(f"ccout_{i}", [512, width], F32)
            DMA("sp", cin_t.ap(), src_sb, R, [b_in])
            val = i + 1

            def coll(e):
                ins = e.collective_compute("AllGather", ALU.bypass, replica_groups=[[0, 1, 2, 3], [4, 5, 6, 7]],
                                           ins=[cin_t.ap().opt()], outs=[cout_t.ap().opt()])
                ins.then_inc(ccsem)
                e.wait_ge(ccsem, val)
                return e.memset(ccdummy[:, 0:1], 0.0)
            if fake_cc:
                for r in range(4):
                    DMA("sp", cout_t.ap()[r * 128:(r + 1) * 128, :], cin_t.ap(), [b_in], [b_out])
            else:
                S.op("pool", coll, [b_in], [b_out, b_ccd])
            return cout_t.ap(), b_out

        cst = {}
        cstb = {}
        for k, shp in CONST_SHAPES.items():
            if k in ("rcos", "rsin"):
                continue
            cst[k] = sb("k_" + k, shp)
            cstb[k] = S.buf("k_" + k)
            DMA("sp", cst[k][:], cst_d[k], [], [cstb[k]])
        ccdummy = sb("ccdummy", [128, 4]); b_ccd = S.buf()
        identb = sb("identb", [128, 128], BF16); b_identb = S.buf()
        CP("dve", identb[:], cst["ident"][:], [cstb["ident"]], [b_identb])
        sel8 = sb("sel8", [8, 128], BF16); b_sel8 = S.buf()
        cum3 = sb("cum3", [8, 3, BT], BF16); b_cum3 = S.buf()
        sel32 = sb("sel32", [32, 128], BF16); b_sel32 = S.buf()
        segm = sb("segm", [128, 4]); b_segm = S.buf()
        prevs = sb("prevs", [128, 4]); b_prevs = S.buf()
        DMA("sp", segm[:], segm_d, [], [b_segm])
        DMA("sp", prevs[:], prev_d, [], [b_prevs])

        X = sb("X", [128, NT, D]); bX = S.bufs(NT, "X")
        for i in range(NT):
            DMA("sp", X[:, i, :], x_d[i * 128:(i + 1) * 128, :], [], [bX[i]])
        hTf = sb("hTf", [128, 8, SEG], BF16); b_hTf = S.bufs(4, "hTf")
        hTf_flat = hTf[:].rearrange("p a b -> p (a b)")
        carve_off = [0]
        carved = []

        class View:
            def __init__(self, ap):
                self.ap = ap

            def __getitem__(self, idx):
                return self.ap[idx]

        def carve(shape, b=None):
            n = 1
            for s_ in shape[1:]:
                n *= s_
            ap = hTf_flat[0:shape[0], carve_off[0]:carve_off[0] + n]
            carve_off[0] += n
            assert carve_off[0] <= 8 * SEG
            if len(shape) == 3:
                ap = ap.rearrange("p (a b) -> p a b", b=shape[2])
            elif len(shape) == 4:
                ap = ap.rearrange("p (a b c) -> p a b c", b=shape[2], c=shape[3])
            bb = S.buf()
            carved.append(bb)
            return View(ap), bb

        NWB = 2
        wbuf = [sb(f"wbuf{i}", [128, 8, 512], BF16) for i in range(NWB)]; b_wbuf = S.bufs(NWB, "wbuf")
        wrr = [0]
        wbig = sb("wbig", [128, 8192], BF16); b_wgu = S.bufs(2, "wgu")
        wgu = [View(wbig[:, k * 4096:(k + 1) * 4096].rearrange("p (kc c) -> p kc c", c=512)) for k in range(2)]
        wdn = [sb(f"wdn{i}", [128, 2, D], BF16) for i in range(2)]; b_wdn = S.bufs(2, "wdn")
        wout = View(wbig[:, :].rearrange("p (kc c) -> p kc c", c=D))
        wr = sb("wr", [128, 8, 36], BF16); b_wr = S.buf()

        def load_wgroup(src2d, ncols, q="pool", R=()):
            i = wrr[0] % NWB
            wrr[0] += 1
            DMA(q, wbuf[i][:, :, 0:ncols], src2d.rearrange("(kc p) c -> p kc c", p=128), list(R), [b_wbuf[i]])
            return wbuf[i], b_wbuf[i]

        def convert_win(l):
            for r in range(8):
                DMA("pool", win_bf[l].ap()[r * 128:(r + 1) * 128, :], win_d[l][r * 128:(r + 1) * 128, :], [], [b_winbf[l]])

        def load_win(l, g, ncols=None):
            n = GCOLS[g] if ncols is None else ncols
            return load_wgroup(win_bf[l].ap()[:, GOFF[g]:GOFF[g] + n], n, q="sp", R=[b_winbf[l]])

        pmraw = sb("pmraw", [NPM, 128]); b_pmraw = S.buf()
        pmT = sb("pmT", [128, NPM]); b_pmT = S.buf()
        bcr = sb("bcr", [128, NBC]); b_bcr = S.buf()
        dtp = sb("dtp", [8, 2]); b_dtp = S.buf()
        negA = sb("negA", [8, 1]); b_negA = S.buf()
        lbs = sb("lbs", [128, 12]); b_lbs = S.buf()
        cact = sb("cact", [128, 8]); b_cact = S.buf()
        cab = sb("cab", [128, 8, 128], BF16); b_cab = S.buf()
        modT = sb("modT", [128, 32]); b_modT = S.buf()
        s1 = sb("s1", [128, 16]); b_s1 = S.buf()
        gbc = sb("gbc", [128, 2, D]); b_gbc = S.bufs(2, "gbc")

        pA = [ps(f"pA{i}", [128, 512]) for i in range(2)]; b_pA = S.bufs(2, "pA"); pArr = [0]
        pT = [ps(f"pT{i}", [128, 512]) for i in range(2)]; b_pT = S.bufs(2, "pT"); pTrr = [0]
        pTRb = ps("pTRb", [128, 1024], BF16); b_pTR = S.buf()
        pSC = ps("pSC", [128, 512]); b_pSC = S.buf()
        pO = ps("pO", [128, 1024]); b_pO = S.buf()

        b_pO_hi = S.buf("pO_hi")
        for _b in list(b_pA) + list(b_pT) + [b_pTR, b_pSC, b_pO, b_pO_hi]:
            _b.excl = True

        def nextA():
            i = pArr[0] % 2
            pArr[0] += 1
            return pA[i], b_pA[i]

        def nextT():
            i = pTrr[0] % 2
            pTrr[0] += 1
            return pT[i], b_pT[i]

        DMA("sp", cact[:], cT_d, [], [b_cact])
        ACT(cact[:], cact[:], AF.Silu, [b_cact], [b_cact])
        CP("dve", cab[:], cact[:].unsqueeze(2).broadcast_to([128, 8, 128]), [b_cact], [b_cab])

        ssq = sb("ssq", [128, 8]); b_ssq = S.buf()
        xn = sb("xn", [128, D], BF16); b_xn = S.buf()
        hT, b_hT = carve([128, 8, BT])
        rawx = sb("rawx", [128, 6, 3 + BT]); b_rawx = S.buf()
        ctmp = sb("ctmp", [128, BT]); b_ctmp = S.buf()
        xbc, b_xbc = carve([128, 6, BT])
        dtf = sb("dtf", [8, 3, BT]); b_dtf = S.buf()
        dttok = sb("dttok", [128, TB, 40]); b_dttok = S.buf()
        ecr = sb("ecr", [128, BT]); b_ecr = S.buf()
        _Cpj = [carve([128, 128]) for _ in range(2)]; Cpj = [a for a, _ in _Cpj]; b_Cpj = [b for _, b in _Cpj]
        dendb = sb("dendb", [128, TB, 8]); b_dendb = S.buf()
        dm = sb("dm", [128, 128]); b_dm = S.buf()
        _sT = [carve([128, 128]) for i in range(2)]; sT = [a for a, _ in _sT]; b_sT = [b for _, b in _sT]; sTrr = [0]
        xs1, b_xs1 = carve([128, 512])
        xs2, b_xs2 = carve([128, 512])
        xsd = sb("xsd", [128, 512]); b_xsd = S.buf()
        Btok, b_Btok = carve([128, 128])
        Sssd = sb("Sssd", [128, 512]); b_Sssd = S.buf()
        Sssdb, b_Sssdb = carve([128, 512])
        Dtot = sb("Dtot", [128, 8]); b_Dtot = S.buf()
        hsg = sb("hsg", [128, 2, BT]); b_hsg = S.buf()
        hlf = sb("hlf", [128, 2, BT]); b_hlf = S.buf()
        hcum = sb("hcum", [128, 2, BT]); b_hcum = S.buf()
        hrc = sb("hrc", [128, 2, BT]); b_hrc = S.buf()
        cres = View(hrc[0:8, :, :]); b_cres = b_hrc
        kend, b_kend = carve([128, 2, BT])
        qp, b_qp = carve([128, 2, BT])
        qpm, b_qpm = carve([128, 2, TB, 64])
        kp, b_kp = carve([128, 2, BT])
        ecend = sb("ecend", [128, 2, BT // 32]); b_ecend = S.buf()
        Lsum = sb("Lsum", [128, 4]); b_Lsum = S.buf()
        kendt, b_kendt = carve([128, 256])
        kendm, b_kendm = carve([128, 256])
        _hv = [carve([128, 256]) for _ in range(TB)]; hvs = [a for a, _ in _hv]; b_hvs = [b for _, b in _hv]
        Sh = [sb(f"Sh{i}", [128, 128]) for i in range(2)]; b_Sh = S.bufs(2, "Sh")
        _Shs = [[carve([128, 128]) for sc in range(4)] for c in range(2)]
        Shs = [[a for a, _ in row] for row in _Shs]; b_Shs = [[b for _, b in row] for row in _Shs]
        rtab = sb("rtab", [128, 2, BT]); b_rtab = S.buf()
        rt1 = sb("rt1", [128, BT]); b_rt1 = S.buf()
        rt2 = sb("rt2", [128, BT]); b_rt2 = S.buf()
        kr, b_kr = carve([128, 2, BT])
        qr, b_qr = carve([128, 2, BT])
        qrg, b_qrg = carve([128, 2, BT])
        krt, b_krt = carve([128, 256])
        _rv = [carve([128, 256]) for _ in range(TB)]; rvbs = [a for a, _ in _rv]; b_rvbs = [b for _, b in _rv]
        Sr = [sb(f"Sr{i}", [128, 128]) for i in range(2)]; b_Sr = S.bufs(2, "Sr")
        _Srb = [carve([128, 128]) for i in range(2)]; Srb = [a for a, _ in _Srb]; b_Srb = [b for _, b in _Srb]
        gzs = [sb(f"gz{t}", [128, 1088]) for t in range(TB)]; b_gzs = S.bufs(TB, "gz")
        gz, b_gz = gzs[0], b_gzs[0]
        yy = sb("yy", [128, 1088]); b_yy = S.buf()
        nst = sb("nst", [128, 24]); b_nst = S.buf()
        mixed, b_mixed = carve([128, D])
        mT = View(hT.ap[:, :, 0:128]); b_mT = b_hT
        otmp = sb("otmp", [128, 512]); b_otmp = S.buf()
        snd, b_snd = gz, b_gz
        rcv, b_rcv = yy, b_yy
        xt1, b_xt1 = otmp, b_otmp
        halo = sb("halo", [128, 18]); b_halo = S.buf()
        lg_sb = View(ecr[:, 64:100]); b_lg = b_ecr
        rt = View(ecr[:, 0:64]); b_rt = b_ecr
        comb = View(ecr[:, 128:160]); b_comb = b_ecr
        combT = View(gzs[0][0:32, :].bitcast(BF16)[:, 0:SEG]); b_combT = [b_gzs[0]] * 4
        cbs = View(yy[:, 0:512]); b_cbs = S.buf()
        sgs = View(yy[:, 512:1024]); b_sgs = S.buf()
        tmo, b_tmo = otmp, b_otmp
        modg, b_modg = cbs, b_cbs
        badab, b_badab = sgs, b_sgs
        hids = [View(gzs[1][:, :].bitcast(BF16)[:, k * 1024:(k + 1) * 1024].rearrange("p (a b) -> p a b", b=512)) for k in range(2)]
        b_hids = S.bufs(2, "hid")
        hidrr = [0]

        MS("pool", qpm[:], 0.0, [b_qpm])
        gm = sb("gm", [128, 2]); b_gm = S.buf()
        MS("dve", gm[:], 0.0, [b_gm])
        MS("dve", gm[0:64, 0:1], 1.0, [b_gm])
        MS("dve", gm[64:128, 1:2], 1.0, [b_gm])
        _Cm = [carve([128, BT]) for _ in range(2)]; Cm = [a for a, _ in _Cm]; b_Cm = [b for _, b in _Cm]
        sel8all, b_sel8all = carve([8, 8, 128])
        qp0, b_qp0 = carve([128, 2, 128])
        qr0, b_qr0 = carve([128, 2, 128])
        qrg0, b_qrg0 = carve([128, 2, 128])
        qpm0, b_qpm0 = carve([128, 2, 64])

        dbg_outs = []

        def rstd_from_ssq(col, n, scale):
            TS("dve", ssq[:, col:col + n], ssq[:, col:col + n], scale, EPS, ALU.mult, ALU.add, [b_ssq], [b_ssq])
            ACT(ssq[:, col:col + n], ssq[:, col:col + n], AF.Sqrt, [b_ssq], [b_ssq])
            RECIP(ssq[:, col:col + n], ssq[:, col:col + n], [b_ssq], [b_ssq])

        def norm_to_hT(i, dst, dst_b, toff, s1col, shcol):
            ACT(xn[:], X[:, i, :], AF.Square, [bX[i]], [b_xn, b_ssq], accum=ssq[:, 0:1])
            rstd_from_ssq(0, 1, 1.0 / D)
            TS("dve", xn[:], X[:, i, :], ssq[:, 0:1], None, ALU.mult, None, [bX[i], b_ssq], [b_xn])
            for j in range(8):
                TR(pTRb[:, j * 128:(j + 1) * 128], xn[:, j * 128:(j + 1) * 128], identb[:], [b_xn, b_identb], [b_pTR])
            for j in range(8):
                ACT(dst[:, j, toff:toff + 128], pTRb[:, j * 128:(j + 1) * 128], AF.Identity,
                    [b_pTR, b_s1, b_modT], [dst_b], bias=modT[:, shcol + j:shcol + j + 1], scale=s1[:, s1col + j:s1col + j + 1])

        def layer_params(l):
            DMA("sp", pmraw[:], pm_d[l], [], [b_pmraw])
            pa, bpa = nextA()
            TR(pa[:, 0:NPM], pmraw[:], cst["ident"][0:NPM, 0:NPM], [b_pmraw, cstb["ident"]], [bpa])
            CP("dve", pmT[:], pa[:, 0:NPM], [bpa], [b_pmT])
            DMA("sp", bcr[:], bc_d[l:l + 1, :].broadcast_to([128, NBC]), [], [b_bcr])
            DMA("sp", dtp[:], dtp_d[l], [], [b_dtp])
            ACT(negA[:], dtp[:, 1:2], AF.Exp, [b_dtp], [b_negA])
            TS("dve", negA[:], negA[:], -1.0, None, ALU.mult, None, [b_negA], [b_negA])
            ACT(lbs[:, 4:12], pmT[:, 46:54], AF.Exp, [b_pmT], [b_lbs])
            for c in range(2):
                TT("dve", lbs[:, 2 + c:3 + c], lbs[:, 4 + c:5 + c], lbs[:, 6 + c:7 + c], ALU.add, [b_lbs], [b_lbs])
                TT("dve", lbs[:, 2 + c:3 + c], lbs[:, 2 + c:3 + c], lbs[:, 8 + c:9 + c], ALU.add, [b_lbs], [b_lbs])
                TT("dve", lbs[:, 2 + c:3 + c], lbs[:, 2 + c:3 + c], lbs[:, 10 + c:11 + c], ALU.add, [b_lbs], [b_lbs])
                RECIP(lbs[:, 2 + c:3 + c], lbs[:, 2 + c:3 + c], [b_lbs], [b_lbs])
                MS("dve", lbs[:, c:c + 1], 0.0, [b_lbs])
                for lp in range(1, l + 1):
                    TT("dve", lbs[:, c:c + 1], lbs[:, c:c + 1], lbs[:, 4 + 2 * lp + c:5 + 2 * lp + c], ALU.add, [b_lbs], [b_lbs])
                TT("dve", lbs[:, c:c + 1], lbs[:, c:c + 1], lbs[:, 2 + c:3 + c], ALU.mult, [b_lbs], [b_lbs])
                TS("dve", lbs[:, 2 + c:3 + c], lbs[:, c:c + 1], -1.0, 1.0, ALU.mult, ALU.add, [b_lbs], [b_lbs])
            for g in range(12):
                wb, bwb = load_wgroup(wada_d[l][:, g * 512:(g + 1) * 512], 512)
                DMA("sp", badab[:], bada_d[l:l + 1, g * 512:(g + 1) * 512].broadcast_to([128, 512]), [], [b_badab])
                pa, bpa = nextA()
                for kc in range(8):
                    MM(pa[:], cab[:, kc, :], wb[:, kc, :], kc == 0, kc == 7, [b_cab, bwb], [bpa])
                which = g // 2
                half = g % 2
                if which in (2, 5):
                    gi = 0 if which == 2 else 1
                    TT("dve", gbc[:, gi, half * 512:(half + 1) * 512], pa[:], badab[:], ALU.add, [bpa, b_badab], [b_gbc[gi]])
                else:
                    TT("dve", modg[:], pa[:], badab[:], ALU.add, [bpa, b_badab], [b_modg])
                    base = {0: 0, 1: 8, 3: 16, 4: 24}[which] + half * 4
                    pt_, bpt_ = nextT()
                    for j in range(4):
                        TR(pt_[:, j * 128:(j + 1) * 128], modg[:, j * 128:(j + 1) * 128], cst["ident"][:], [b_modg, cstb["ident"]], [bpt_])
                    CP("dve", modT[:, base:base + 4], pt_[:, 0:512:128], [bpt_], [b_modT])
            TS("dve", s1[:, 0:8], modT[:, 8:16], 1.0, None, ALU.add, None, [b_modT], [b_s1])
            TT("dve", s1[:, 0:8], s1[:, 0:8], pmT[:, 0:8], ALU.mult, [b_s1, b_pmT], [b_s1])
            TS("dve", s1[:, 8:16], modT[:, 24:32], 1.0, None, ALU.add, None, [b_modT], [b_s1])
            TT("dve", s1[:, 8:16], s1[:, 8:16], pmT[:, 8:16], ALU.mult, [b_s1, b_pmT], [b_s1])

        def inproj_F(l, g, chunks, consume):
            wb, bwb = load_win(l, g)
            for ci in chunks:
                pa, bpa = nextA()
                for kc in range(8):
                    MM(pa[:, 0:BT], wb[:, kc, ci * 128:(ci + 1) * 128], hT[:, kc, :], kc == 0, kc == 7, [bwb, b_hT], [bpa])
                consume(ci, pa, bpa)

        def inproj_T(l, g, consume):
            wb, bwb = load_win(l, g)
            for t in range(TB):
                pt_, bpt_ = nextT()
                for kc in range(8):
                    MM(pt_[:, 0:GCOLS[g]], hT[:, kc, t * 128:(t + 1) * 128], wb[:, kc, 0:GCOLS[g]], kc == 0, kc == 7, [bwb, b_hT], [bpt_])
                consume(t, pt_, bpt_)

        def mixer_block(l, blk, full):
            t0 = blk * TB
            for t in range(TB):
                norm_to_hT(t0 + t, hT, b_hT, t * 128, 0, 0)
            if blk > 0:
                CP("pool", rawx[:, :, 0:3], rawx[:, :, BT:BT + 3], [b_rawx], [b_rawx])
            else:
                CP("pool", rawx[:, :, 0:3], halo[:].rearrange("p (c k) -> p c k", k=3), [b_halo, b_rawx], [b_rawx])

            def conv_chunk(c, pa, bpa):
                ACT(rawx[:, c, 3:3 + BT], pa[:, 0:BT], AF.Copy, [bpa], [b_rawx])
                TS("dve", ctmp[:], rawx[:, c, 0:BT], pmT[:, 16 + c * 4:17 + c * 4], pmT[:, 40 + c:41 + c], ALU.mult, ALU.add,
                   [b_rawx, b_pmT], [b_ctmp])
                for j in range(1, 4):
                    STT(ctmp[:], rawx[:, c, j:j + BT], pmT[:, 16 + c * 4 + j:17 + c * 4 + j], ctmp[:], ALU.mult, ALU.add,
                        [b_rawx, b_pmT, b_ctmp], [b_ctmp])
                ACT(xbc[:, c, :], ctmp[:], AF.Silu, [b_ctmp], [b_xbc])

            inproj_F(l, G_XS, range(4), conv_chunk)
            if CUT <= 1:
                return

            def bcf_chunk(ci, pa, bpa):
                if ci < 2:
                    conv_chunk(4 + ci, pa, bpa)
                else:
                    c = ci - 2
                    ACT(hsg[:, c, :], pa[:, 0:BT], AF.Sigmoid, [bpa], [b_hsg])
            inproj_F(l, G_BCF, range(4), bcf_chunk)

            for c in range(2):
                TS("dve", hsg[:, c, :], hsg[:, c, :], lbs[:, 2 + c:3 + c], lbs[:, c:c + 1], ALU.mult, ALU.add, [b_hsg, b_lbs], [b_hsg])
                TS("dve", hlf[:, c, :], hsg[:, c, :], 1e-30, None, ALU.max, None, [b_hsg], [b_hlf])
            ACT(hlf[:], hlf[:], AF.Ln, [b_hlf], [b_hlf])
            TS("dve", hsg[:], hsg[:], -1.0, 1.0, ALU.mult, ALU.add, [b_hsg], [b_hsg])
            for c in range(2):
                SCAN(hcum[:, c, :], cst["reset32"][:], hlf[:, c, :], [cstb["reset32"], b_hlf], [b_hcum])
            NSUB = BT // 32
            for c in range(2):
                cv = hcum[:, c, :].rearrange("p (s k) -> p s k", k=32)
                TT("dve", hrc[:, c, :].rearrange("p (s k) -> p s k", k=32), cv[:, :, 31:32].broadcast_to([128, NSUB, 32]), cv,
                   ALU.subtract, [b_hcum], [b_hrc])
            ACT(hrc[:], hrc[:], AF.Exp, [b_hrc], [b_hrc])
            TT("dve", kend[:], hsg[:], hrc[:], ALU.mult, [b_hsg, b_hrc], [b_kend])
            for c in range(2):
                ACT(ecend[:, c, :], hcum[:, c, 31:BT:32], AF.Exp, [b_hcum], [b_ecend])
                if not full:
                    RED(Lsum[:, 2 + c:3 + c], hcum[:, c, 31:BT:32], ALU.add, [b_hcum], [b_Lsum])
                    TT("dve", Lsum[:, c:c + 1], Lsum[:, c:c + 1], Lsum[:, 2 + c:3 + c], ALU.add, [b_Lsum], [b_Lsum])

            if CUT <= 2:
                return
            DMA("sp", rtab[:, 0, :], cst_d["rcos"][:, blk * BT:(blk + 1) * BT], [], [b_rtab])
            DMA("sp", rtab[:, 1, :], cst_d["rsin"][:, blk * BT:(blk + 1) * BT], [], [b_rtab])

            def rot_group(dst, dst_b):
                def f(ci, pa, bpa):
                    if ci < 2:
                        TT("dve", (rt1 if ci == 0 else rt2)[:], pa[:, 0:BT], rtab[:, 0, :], ALU.mult, [bpa, b_rtab],
                           [b_rt1 if ci == 0 else b_rt2])
                    else:
                        c = ci - 2
                        src, bsrc = (rt1, b_rt1) if c == 0 else (rt2, b_rt2)
                        STT(dst[:, c, :], pa[:, 0:BT], 1.0, rtab[:, 1, :], ALU.mult, ALU.mult, [bpa, b_rtab], [dst_b])
                        TT("dve", dst[:, c, :], dst[:, c, :], src[:], ALU.add, [dst_b, bsrc], [dst_b])
                return f
            inproj_F(l, G_RK, range(4), rot_group(kr, b_kr))

            def dt_chunk(ci, pa, bpa):
                ACT(dtf[:, 0, :], pa[0:8, 0:BT], AF.Exp, [bpa, b_dtp], [b_dtf], bias=dtp[:, 0:1])
                ACT(dtf[:, 0, :], dtf[:, 0, :], AF.Ln, [b_dtf], [b_dtf], bias=1.0)
                TS("dve", dtf[:, 1, :], dtf[:, 0, :], negA[:, 0:1], None, ALU.mult, None, [b_dtf, b_negA], [b_dtf])
                SCAN(dtf[:, 2, :], cst["reset128"][0:8, :], dtf[:, 1, :], [cstb["reset128"], b_dtf], [b_dtf])
                CP("dve", cum3[:, 0, :], dtf[:, 2, :], [b_dtf], [b_cum3])
                TT("dve", cres[:, 0, :], dtf[:, 2, :], cum3[:, 0, :], ALU.subtract, [b_dtf, b_cum3], [b_cres])
                CP("dve", cum3[:, 1, :], cres[:, 0, :], [b_cres], [b_cum3])
                TT("dve", cres[:, 1, :], cres[:, 0, :], cum3[:, 1, :], ALU.subtract, [b_cres, b_cum3], [b_cres])
                CP("dve", cum3[:, 2, :], cres[:, 1, :], [b_cres], [b_cum3])
            wb, bwb = load_win(l, G_DT, 8)
            pa, bpa = nextA()
            for kc in range(8):
                MM(pa[0:8, 0:BT], wb[:, kc, 0:8], hT[:, kc, :], kc == 0, kc == 7, [bwb, b_hT], [bpa])
            dt_chunk(0, pa, bpa)
            for t in range(TB):
                pt_, bpt_ = nextT()
                TR(pt_[:, 0:8], dtf[:, 0, t * 128:(t + 1) * 128], cst["ident"][0:8, 0:8], [b_dtf, cstb["ident"]], [bpt_])
                TR(pt_[:, 8:16], dtf[:, 2, t * 128:(t + 1) * 128], cst["ident"][0:8, 0:8], [b_dtf, cstb["ident"]], [bpt_])
                CP("dve", dttok[:, t, 0:16], pt_[:, 0:16], [bpt_], [b_dttok])

            if CUT <= 3:
                return
            def hirv(t, pt_, bpt_):
                pass
            def hirv_c(t, pt_, bpt_):
                CP("act", hvs[t][:], pt_[:, 0:256], [bpt_], [b_hvs[t]])
                CP("dve", rvbs[t][:], pt_[:, 256:512], [bpt_], [b_rvbs[t]])
            inproj_T(l, G_HIRV, hirv_c)
            if CUT == 35:
                return
            if full:
                def hqrq_chunk(ci, pa, bpa):
                    if ci < 2:
                        ACT(hrc[:, ci, :], hcum[:, ci, :], AF.Exp, [b_hcum, b_hrc], [b_hrc])
                        TT("dve", qp[:, ci, :], pa[:, 0:BT], hrc[:, ci, :], ALU.mult, [bpa, b_hrc], [b_qp])
                        ACT(hrc[:, ci, :], hcum[:, ci, :], AF.Exp, [b_hcum, b_hrc], [b_hrc], scale=-1.0)
                        TT("dve", kp[:, ci, :], hsg[:, ci, :], hrc[:, ci, :], ALU.mult, [b_hsg, b_hrc], [b_kp])
                        for t in range(TB):
                            CP("pool", qpm[:, ci, t, 32:64], qp[:, ci, t * 128 + 96:t * 128 + 128], [b_qp], [b_qpm])
                    else:
                        c = ci - 2
                        TT("dve", (rt1 if c == 0 else rt2)[:], pa[:, 0:BT], rtab[:, 0, :], ALU.mult, [bpa, b_rtab],
                           [b_rt1 if c == 0 else b_rt2])
                inproj_F(l, G_HQRQ, range(4), hqrq_chunk)

                def rqs_chunk(c, pa, bpa):
                    src, bsrc = (rt1, b_rt1) if c == 0 else (rt2, b_rt2)
                    STT(qr[:, c, :], pa[:, 0:BT], 1.0, rtab[:, 1, :], ALU.mult, ALU.mult, [bpa, b_rtab], [b_qr])
                    TT("dve", qr[:, c, :], qr[:, c, :], src[:], ALU.add, [b_qr, bsrc], [b_qr])
                    for t in range(TB):
                        TT("pool", qrg[:, c, t * 128:(t + 1) * 128], qr[:, c, t * 128:(t + 1) * 128], cst["gq"][:, c, :], ALU.mult,
                           [b_qr, cstb["gq"]], [b_qrg])
                inproj_F(l, G_RQS, range(2), rqs_chunk)
                def z_c(t, pt_, bpt_):
                    ACT(gzs[t][:, 0:512], pt_[:, 0:512], AF.Silu, [bpt_], [b_gzs[t]])
                inproj_T(l, G_Z, z_c)

                def hgrg_c(t, pt_, bpt_):
                    ACT(gzs[t][:, 512:768], pt_[:, 0:256], AF.Sigmoid, [bpt_], [b_gzs[t]])
                    ACT(gzs[t][:, 768:1024], pt_[:, 256:512], AF.Silu, [bpt_], [b_gzs[t]])
                inproj_T(l, G_HGRG, hgrg_c)

            if full:
                for g in range(2):
                    TS("dve", Cm[g][:], xbc[:, 5, :], gm[:, g:g + 1], None, ALU.mult, None, [b_xbc, b_gm], [b_Cm[g]])
            for h in range(8):
                g = h // 4
                pa, bpa = nextA()
                for q in range(3):
                    MM(pa[:, 0:BT], sel8all[:, h, :], cum3[:, q, :], q == 0, q == 2, [b_sel8all, b_cum3], [bpa])
                ACT(ecr[:], pa[:, 0:BT], AF.Exp, [bpa], [b_ecr])
                for t in range(TB):
                    CP("dve", dendb[:, t, h:h + 1], ecr[:, t * 128 + 127:t * 128 + 128], [b_ecr], [b_dendb])
                    CP("dve", dttok[:, t, 16 + h:17 + h], pa[:, t * 128 + 127:t * 128 + 128], [bpa], [b_dttok])

            if CUT <= 4 or CUT == 35:
                return
            for t in range(TB):
                i = t0 + t
                tsl = slice(t * 128, (t + 1) * 128)
                TT("dve", dttok[:, t, 24:32], dttok[:, t, 16:24], dttok[:, t, 8:16], ALU.subtract, [b_dttok], [b_dttok])
                ACT(dttok[:, t, 24:32], dttok[:, t, 24:32], AF.Exp, [b_dttok], [b_dttok])
                TT("dve", dttok[:, t, 32:40], dttok[:, t, 24:32], dttok[:, t, 0:8], ALU.mult, [b_dttok], [b_dttok])
                for c in range(4):
                    TR(pTRb[:, c * 128:(c + 1) * 128], xbc[:, c, tsl], identb[:], [b_xbc, b_identb], [b_pTR])
                TR(pTRb[:, 512:640], xbc[:, 4, tsl], identb[:], [b_xbc, b_identb], [b_pTR])
                xv = pTRb[:, 0:512].rearrange("p (h k) -> p h k", k=64)
                TT("dve", xs2[:].rearrange("p (h k) -> p h k", k=64), xv, dttok[:, t, 32:40].unsqueeze(2).broadcast_to([128, 8, 64]),
                   ALU.mult, [b_pTR, b_dttok], [b_xs2])
                if full:
                    TT("dve", xs1[:].rearrange("p (h k) -> p h k", k=64), xv, dttok[:, t, 0:8].unsqueeze(2).broadcast_to([128, 8, 64]),
                       ALU.mult, [b_pTR, b_dttok], [b_xs1])
                    TT("dve", xsd[:].rearrange("p (h k) -> p h k", k=64), xv, bcr[:, 768:776].unsqueeze(2).broadcast_to([128, 8, 64]),
                       ALU.mult, [b_pTR, b_bcr], [b_xsd])
                CP("act", Btok[:], pTRb[:, 512:640], [b_pTR], [b_Btok])
                hv, b_hv, rvb, b_rvb = hvs[t], b_hvs[t], rvbs[t], b_rvbs[t]
                for c in range(2):
                    TR(pTRb[:, c * 128:(c + 1) * 128], kend[:, c, tsl], identb[:], [b_kend, b_identb], [b_pTR])
                    TR(pTRb[:, 256 + c * 128:256 + (c + 1) * 128], kr[:, c, tsl], identb[:], [b_kr, b_identb], [b_pTR])
                CP("act", kendt[:], pTRb[:, 0:256], [b_pTR], [b_kendt])
                TS("dve", kendm[64:128, :], pTRb[64:128, 0:256], cst["mask96"][64:128, 0:1], None, ALU.mult, None,
                   [b_pTR, cstb["mask96"]], [b_kendm])
                TT("dve", krt[:].rearrange("p (h k) -> p h k", k=64), pTRb[:, 256:512].rearrange("p (h k) -> p h k", k=64),
                   cst["gk"][:].unsqueeze(2).broadcast_to([128, 4, 64]), ALU.mult, [b_pTR, cstb["gk"]], [b_krt])

                for sc in range(4):
                    sub = t * 4 + sc
                    for c in range(2):
                        if full:
                            CP("pool", Shs[c][sc][:], Sh[c][:], [b_Sh[c]], [b_Shs[c][sc]])
                        pa, bpa = nextA()
                        cs = slice(c * 128, (c + 1) * 128)
                        if sc < 3:
                            MM(pa[:, 0:128], kendt[sc * 32:(sc + 1) * 32, cs], hv[sc * 32:(sc + 1) * 32, cs], True, True, [b_kendt, b_hv], [bpa])
                        else:
                            MM(pa[:, 0:128], kendm[64:128, cs], hv[64:128, cs], True, True, [b_kendm, b_hv], [bpa])
                        STT(Sh[c][:], Sh[c][:], ecend[:, c, sub:sub + 1], pa[:, 0:128], ALU.mult, ALU.add, [b_Sh[c], b_ecend, bpa], [b_Sh[c]])

                if full and CUT2 >= 3:
                    for c in range(2):
                        TS("dve", qp0[:, c, :], qp[:, c, tsl], gm[:, 0:1], None, ALU.mult, None, [b_qp, b_gm], [b_qp0])
                        TS("dve", qr0[:, c, :], qr[:, c, tsl], gm[:, 0:1], None, ALU.mult, None, [b_qr, b_gm], [b_qr0])
                        TS("dve", qrg0[:, c, :], qrg[:, c, tsl], gm[:, 0:1], None, ALU.mult, None, [b_qrg, b_gm], [b_qrg0])
                        TS("dve", qpm0[:, c, :], qpm[:, c, t, :], gm[:, 0:1], None, ALU.mult, None, [b_qpm, b_gm], [b_qpm0])
                    for g in range(2):
                        MM(pSC[:, g * 128:(g + 1) * 128], xbc[:, 4, tsl], Cm[g][:, tsl], True, True, [b_xbc, b_Cm[g]], [b_pSC])
                    for h in range(8 if CUT2 != 313 else 0):
                        g = h // 4
                        k = sTrr[0] % 2; sTrr[0] += 1
                        pa, bpa = nextA()
                        for q in range(3):
                            MM(pa[:, 0:128], sel8all[:, h, :], cum3[:, q, tsl], q == 0, q == 2, [b_sel8all, b_cum3], [bpa])
                        STT(dm[:], pa[:, 0:128], dttok[:, t, 8 + h:9 + h], cst["negmask"][:], ALU.subtract, ALU.min,
                            [bpa, b_dttok, cstb["negmask"]], [b_dm])
                        ACT(ecr[:, 0:128], pa[:, 0:128], AF.Exp, [bpa], [b_ecr])
                        TT("dve", Cpj[k][:], Cm[g][:, tsl], ecr[:, 0:128], ALU.mult, [b_Cm[g], b_ecr], [b_Cpj[k]])
                        ACT(dm[:], dm[:], AF.Exp, [b_dm], [b_dm])
                        TT("dve", sT[k][:], pSC[:, g * 128:(g + 1) * 128], dm[:], ALU.mult, [b_pSC, b_dm], [b_sT[k]])
                        if CUT2 == 311:
                            continue
                        MM(pO[:, h * 64:(h + 1) * 64], sT[k][:], xs1[:, h * 64:(h + 1) * 64], True, CUT2 == 312, [b_sT[k], b_xs1], [b_pO])
                        if CUT2 == 312:
                            continue
                        MM(pO[:, h * 64:(h + 1) * 64], Cpj[k][:], Sssdb[:, h * 64:(h + 1) * 64], False, True, [b_Cpj[k], b_Sssdb], [b_pO])
                    for h in range(4 if CUT2 not in (31, 311, 312, 313) else 0):
                        c = h // 2
                        hr = slice((h % 2) * 64, (h % 2) * 64 + 64)
                        ev = (h % 2 == 0)
                        if ev:
                            MM(pSC[:, 256:384], kp[:, c, tsl], qp0[:, c, :], True, True, [b_kp, b_qp0], [b_pSC])
                        else:
                            MM(pSC[:, 256:384], kp[hr, c, tsl], qp[hr, c, tsl], True, True, [b_kp, b_qp], [b_pSC])
                        k = sTrr[0] % 2; sTrr[0] += 1
                        TT("dve", sT[k][:], pSC[:, 256:384], cst["blk32"][:], ALU.mult, [b_pSC, cstb["blk32"]], [b_sT[k]])
                        oc = slice(512 + h * 64, 512 + (h + 1) * 64)
                        for sc in (3, 2, 0, 1):
                            hcols = slice((h % 2) * 64, (h % 2) * 64 + 64)
                            if ev:
                                rhs = Shs[c][sc][:, hcols]
                                if sc < 3:
                                    MM(pO[sc * 32:(sc + 1) * 32, oc], qp0[:, c, sc * 32:(sc + 1) * 32], rhs, sc != 2, False,
                                       [b_qp0, b_Shs[c][sc]], [b_pO])
                                else:
                                    MM(pO[64:128, oc], qpm0[:, c, :], rhs, True, False, [b_qpm0, b_Shs[c][sc]], [b_pO])
                            else:
                                rhs = Shs[c][sc][hr, hcols]
                                if sc < 3:
                                    MM(pO[sc * 32:(sc + 1) * 32, oc], qp[hr, c, t * 128 + sc * 32:t * 128 + (sc + 1) * 32], rhs, sc != 2, False,
                                       [b_qp, b_Shs[c][sc]], [b_pO])
                                else:
                                    MM(pO[64:128, oc], qpm[hr, c, t, :], rhs, True, False, [b_qpm, b_Shs[c][sc]], [b_pO])
                        MM(pO[:, oc], sT[k][:], hv[:, h * 64:(h + 1) * 64], False, True, [b_sT[k], b_hv], [b_pO])
                    for h in range(4 if CUT2 not in (31, 32, 311, 312, 313) else 0):
                        c = h // 2
                        hr = slice((h % 2) * 64, (h % 2) * 64 + 64)
                        ev = (h % 2 == 0)
                        if ev:
                            MM(pSC[:, 384:512], kr[:, c, tsl], qr0[:, c, :], True, True, [b_kr, b_qr0], [b_pSC])
                        else:
                            MM(pSC[:, 384:512], kr[hr, c, tsl], qr[hr, c, tsl], True, True, [b_kr, b_qr], [b_pSC])
                        k = sTrr[0] % 2; sTrr[0] += 1
                        TT("dve", sT[k][:], pSC[:, 384:512], cst["retmask"][:, h, :], ALU.mult, [b_pSC, cstb["retmask"]], [b_sT[k]])
                        oc = slice(768 + h * 64, 768 + (h + 1) * 64)
                        MM(pO[:, oc], sT[k][:], rvb[:, h * 64:(h + 1) * 64], True, False, [b_sT[k], b_rvb], [b_pO])
                        if ev:
                            MM(pO[:, oc], qrg0[:, c, :], Srb[c][:, 0:64], False, True, [b_qrg0, b_Srb[c]], [b_pO])
                        else:
                            MM(pO[:, oc], qrg[hr, c, tsl], Srb[c][hr, 64:128], False, True, [b_qrg, b_Srb[c]], [b_pO])

                pa, bpa = nextA()
                MM(pa[:], Btok[:], xs2[:], True, True, [b_Btok, b_xs2], [bpa])
                TT("dve", Sssd[:].rearrange("p (h k) -> p h k", k=64), Sssd[:].rearrange("p (h k) -> p h k", k=64),
                   dendb[:, t, :].unsqueeze(2).broadcast_to([128, 8, 64]), ALU.mult, [b_Sssd, b_dendb], [b_Sssd])
                TT("dve", Sssd[:], Sssd[:], pa[:], ALU.add, [b_Sssd, bpa], [b_Sssd])
                if full:
                    CP("pool", Sssdb[:], Sssd[:], [b_Sssd], [b_Sssdb])
                if not full:
                    TT("dve", Dtot[:], Dtot[:], dendb[:, t, :], ALU.mult, [b_Dtot, b_dendb], [b_Dtot])
                for c in range(2):
                    pa, bpa = nextA()
                    cs = slice(c * 128, (c + 1) * 128)
                    MM(pa[:, 0:128], krt[:, cs], rvb[:, cs], True, True, [b_krt, b_rvb], [bpa])
                    STT(Sr[c][:], Sr[c][:], cst["g128"][:, c:c + 1], pa[:, 0:128], ALU.mult, ALU.add, [b_Sr[c], cstb["g128"], bpa], [b_Sr[c]])
                    if full:
                        CP("pool", Srb[c][:], Sr[c][:], [b_Sr[c]], [b_Srb[c]])

                if full and CUT2 >= 4 and CUT2 not in (31, 32, 311, 312, 313):
                    finish_tile(l, t, i)
            return


        def finish_tile(l, t, i):
            tsl = slice(t * 128, (t + 1) * 128)
            gz, b_gz = gzs[t], b_gzs[t]
            TT("dve", yy[:, 0:512], pO[:, 0:512], xsd[:], ALU.add, [b_pO, b_xsd], [b_yy])
            TT("dve", yy[:, 0:512], yy[:, 0:512], gz[:, 0:512], ALU.mult, [b_yy, b_gz], [b_yy])
            CP("act", yy[:, 512:1024], pO[:, 512:1024], [b_pO], [b_yy])
            TT("dve", otmp[:], yy[:, 0:512], yy[:, 0:512], ALU.mult, [b_yy], [b_otmp])
            RED(nst[:, 0:8], otmp[:].rearrange("p (g k) -> p g k", k=64), ALU.add, [b_otmp], [b_nst])
            TT("dve", otmp[:], yy[:, 512:1024], yy[:, 512:1024], ALU.mult, [b_yy, b_otmp], [b_otmp])
            RED(nst[:, 8:16], otmp[:].rearrange("p (g k) -> p g k", k=64), ALU.add, [b_otmp], [b_nst])
            RED(nst[:, 16:18], nst[:, 0:8].rearrange("p (g k) -> p g k", k=4), ALU.add, [b_nst], [b_nst])
            TS("dve", nst[:, 16:18], nst[:, 16:18], 1.0 / 256, EPS, ALU.mult, ALU.add, [b_nst], [b_nst])
            TS("dve", nst[:, 8:16], nst[:, 8:16], 1.0 / 64, EPS, ALU.mult, ALU.add, [b_nst], [b_nst])
            ACT(nst[:, 8:18], nst[:, 8:18], AF.Sqrt, [b_nst], [b_nst])
            RECIP(nst[:, 8:18], nst[:, 8:18], [b_nst], [b_nst])
            for g in range(2):
                STT(mixed[:, g * 256:(g + 1) * 256], yy[:, g * 256:(g + 1) * 256], nst[:, 16 + g:17 + g], bcr[:, g * 256:(g + 1) * 256],
                    ALU.mult, ALU.mult, [b_yy, b_nst, b_bcr], [b_mixed])
            TT("dve", yy[:, 512:1024].rearrange("p (h k) -> p h k", k=64), yy[:, 512:1024].rearrange("p (h k) -> p h k", k=64),
               nst[:, 8:16].unsqueeze(2).broadcast_to([128, 8, 64]), ALU.mult, [b_yy, b_nst], [b_yy])
            TT("dve", yy[:, 512:768], yy[:, 512:768], bcr[:, 512:768], ALU.mult, [b_yy, b_bcr], [b_yy])
            TT("dve", mixed[:, 512:1024], yy[:, 512:1024], gz[:, 512:1024], ALU.mult, [b_yy, b_gz], [b_mixed])
            for j in range(8):
                TR(pTRb[:, j * 128:(j + 1) * 128], mixed[:, j * 128:(j + 1) * 128], identb[:], [b_mixed, b_identb], [b_pTR])
            CP("act", mT[:], pTRb[:].rearrange("p (a b) -> p a b", b=128), [b_pTR], [b_mT])
            for half in range(2):
                pa, bpa = nextA()
                for kc in range(8):
                    MM(pa[:], mT[:, kc, :], wout[:, kc, half * 512:(half + 1) * 512], kc == 0, kc == 7, [b_mT, b_wgu[0], b_wgu[1]], [bpa])
                TT("dve", otmp[:], pa[:], gbc[:, 0, half * 512:(half + 1) * 512], ALU.mult, [bpa, b_gbc[0]], [b_otmp])
                TT("dve", X[:, i, half * 512:(half + 1) * 512], X[:, i, half * 512:(half + 1) * 512], otmp[:], ALU.add,
                   [bX[i], b_otmp], [bX[i]])

        def barrier():
            allb = carved + list(b_hTf) + [b_yy, b_cbs, b_sgs, b_gzs[0], b_gzs[1]] + list(b_hids) + [b_pO, b_pO_hi]
            S.op("dve", lambda e: e.memset(ccdummy[:, 1:2], 0.0), [], allb + [b_ccd])

        def reset_states(one_for_dtot=True):
            if stage != 2.7:
                MS("dve", Sssd[:], 0.0, [b_Sssd])
            for c in range(2):
                if stage != 2.7:
                    MS("dve", Sh[c][:], 0.0, [b_Sh[c]])
                    MS("dve", Sr[c][:], 0.0, [b_Sr[c]])
            if stage != 2.7:
                MS("dve", Dtot[:], 1.0, [b_Dtot])
                MS("dve", Lsum[:], 0.0, [b_Lsum])
            MS("pool", qpm[:], 0.0, [b_qpm])
            for h in range(8):
                CP("dve", sel8all[:, h, :], identb[0:8, h:h + 1].broadcast_to([8, 128]), [b_identb], [b_sel8all])

        def halo_exchange(l):
            for t in range(TB):
                norm_to_hT(NT - TB + t, hT, b_hT, t * 128, 0, 0)

            MS("dve", snd[:, 0:256], 0.0, [b_snd])

            def grab(base):
                def f(ci, pa, bpa):
                    c = base + ci
                    CP("dve", snd[:, c * 3:(c + 1) * 3], pa[:, BT - 3:BT], [bpa], [b_snd])
                return f
            inproj_F(l, G_XS, range(4), grab(0))
            inproj_F(l, G_BCF, range(2), grab(4))
            if stage == 2:
                return
            gath, b_g = allgather(snd[:, 0:256], 256, [b_snd], None)
            if stage == 2.2:
                return
            MS("dve", halo[:], 0.0, [b_halo])
            for r in range(4):
                DMA("sp", rcv[:, 0:18], gath[r * 128:(r + 1) * 128, 0:18], [b_g], [b_rcv])
                STT(halo[:], rcv[:, 0:18], prevs[:, r:r + 1], halo[:], ALU.mult, ALU.add, [b_rcv, b_prevs, b_halo], [b_halo])

        def state_exchange():
            CP("dve", snd[:, 0:512], Sssd[:], [b_Sssd], [b_snd])
            for c in range(2):
                CP("dve", snd[:, 512 + c * 128:640 + c * 128], Sh[c][:], [b_Sh[c]], [b_snd])
                CP("dve", snd[:, 768 + c * 128:896 + c * 128], Sr[c][:], [b_Sr[c]], [b_snd])
            CP("dve", snd[:, 1024:1032], Dtot[:], [b_Dtot], [b_snd])
            ACT(snd[:, 1032:1034], Lsum[:, 0:2], AF.Exp, [b_Lsum], [b_snd])
            MS("dve", snd[:, 1034:NS], 0.0, [b_snd])
            gath, b_g = allgather(snd[:, 0:NS], NS, [b_snd], None)
            MS("dve", Sssd[:], 0.0, [b_Sssd])
            for c in range(2):
                MS("dve", Sh[c][:], 0.0, [b_Sh[c]])
                MS("dve", Sr[c][:], 0.0, [b_Sr[c]])
            for r in range(4):
                DMA("sp", rcv[:, 0:NS], gath[r * 128:(r + 1) * 128, :], [b_g], [b_rcv])
                m = segm[:, r:r + 1]
                v3 = lambda a: a.rearrange("p (h k) -> p h k", k=64)
                TT("dve", v3(xt1[:]), v3(Sssd[:]), rcv[:, 1024:1032].unsqueeze(2).broadcast_to([128, 8, 64]), ALU.mult,
                   [b_Sssd, b_rcv], [b_xt1])
                TT("dve", xt1[:], xt1[:], rcv[:, 0:512], ALU.add, [b_xt1, b_rcv], [b_xt1])
                TT("dve", xt1[:], xt1[:], Sssd[:], ALU.subtract, [b_xt1, b_Sssd], [b_xt1])
                STT(Sssd[:], xt1[:], m, Sssd[:], ALU.mult, ALU.add, [b_xt1, b_segm, b_Sssd], [b_Sssd])
                for c in range(2):
                    for (St, bSt, off, sc_ap, sc_b) in ((Sh[c], b_Sh[c], 512 + c * 128, rcv[:, 1032 + c:1033 + c], b_rcv),
                                                       (Sr[c], b_Sr[c], 768 + c * 128, cst["g2048"][:, c:c + 1], cstb["g2048"])):
                        STT(xt1[:, 0:128], St[:], sc_ap, rcv[:, off:off + 128], ALU.mult, ALU.add, [bSt, sc_b, b_rcv], [b_xt1])
                        TT("dve", xt1[:, 0:128], xt1[:, 0:128], St[:], ALU.subtract, [b_xt1, bSt], [b_xt1])
                        STT(St[:], xt1[:, 0:128], m, St[:], ALU.mult, ALU.add, [b_xt1, b_segm, bSt], [bSt])
            CP("pool", Sssdb[:], Sssd[:], [b_Sssd], [b_Sssdb])
            for c in range(2):
                CP("pool", Srb[c][:], Sr[c][:], [b_Sr[c]], [b_Srb[c]])

        def moe(l):
            DMA("pool", wr[:], wr_d[l].rearrange("(kc p) c -> p kc c", p=128), [], [b_wr])
            for i in range(NT):
                norm_to_hT(i, hTf, b_hTf[i // 4], i * 128, 8, 16)
                pa, bpa = nextA()
                for kc in range(8):
                    MM(pa[:, 0:36], hTf[:, kc, i * 128:(i + 1) * 128], wr[:, kc, :], kc == 0, kc == 7, [b_hTf[i // 4], b_wr], [bpa])
                TT("dve", lg_sb[:], pa[:, 0:36], bcr[:, 776:812], ALU.add, [bpa, b_bcr], [b_lg])
                R_, W_ = [b_lg, b_rt], [b_rt]
                RED(rt[:, 0:1], lg_sb[:, 0:4], ALU.max, R_, W_)
                TS("dve", rt[:, 4:8], lg_sb[:, 0:4], rt[:, 0:1], None, ALU.is_equal, None, R_, W_)
                TS("dve", rt[:, 1:2], rt[:, 0:1], -1.0, None, ALU.mult, None, R_, W_)
                ACT(rt[:, 8:12], lg_sb[:, 0:4], AF.Exp, R_, W_, bias=rt[:, 1:2], accum=rt[:, 2:3])
                RECIP(rt[:, 3:4], rt[:, 2:3], R_, W_)
                TT("dve", rt[:, 16:48].rearrange("p (g e) -> p g e", e=8), lg_sb[:, 4:36].rearrange("p (g e) -> p g e", e=8),
                   rt[:, 4:8].unsqueeze(2).broadcast_to([128, 4, 8]), ALU.mult, R_, W_)
                RED(rt[:, 48:56], rt[:, 16:48].rearrange("p (g e) -> p e g", e=8), ALU.add, R_, W_)
                RED(rt[:, 56:57], rt[:, 48:56], ALU.max, R_, W_)
                TS("dve", rt[:, 16:24], rt[:, 48:56], rt[:, 56:57], None, ALU.is_equal, None, R_, W_)
                STT(rt[:, 24:32], rt[:, 16:24], NEG, rt[:, 48:56], ALU.mult, ALU.add, R_, W_)
                RED(rt[:, 57:58], rt[:, 24:32], ALU.max, R_, W_)
                TS("dve", rt[:, 32:40], rt[:, 24:32], rt[:, 57:58], None, ALU.is_equal, None, R_, W_)
                TT("dve", rt[:, 58:59], rt[:, 56:57], rt[:, 57:58], ALU.subtract, R_, W_)
                ACT(rt[:, 59:60], rt[:, 58:59], AF.Sigmoid, R_, W_)
                TT("dve", rt[:, 60:61], rt[:, 59:60], rt[:, 3:4], ALU.mult, R_, W_)
                TT("dve", rt[:, 61:62], rt[:, 3:4], rt[:, 60:61], ALU.subtract, R_, W_)
                TS("dve", rt[:, 40:48], rt[:, 16:24], rt[:, 60:61], None, ALU.mult, None, R_, W_)
                STT(rt[:, 40:48], rt[:, 32:40], rt[:, 61:62], rt[:, 40:48], ALU.mult, ALU.add, R_, W_)
                TT("dve", comb[:].rearrange("p (g e) -> p g e", e=8), rt[:, 4:8].unsqueeze(2).broadcast_to([128, 4, 8]),
                   rt[:, 40:48].unsqueeze(1).broadcast_to([128, 4, 8]), ALU.mult, [b_rt], [b_comb])
                pt_, bpt_ = nextT()
                TR(pt_[0:32, 0:128], comb[:], cst["ident"][:], [b_comb, cstb["ident"]], [bpt_])
                CP("act", combT[:, i * 128:(i + 1) * 128], pt_[0:32, 0:128], [bpt_], [b_combT[i // 4]])
            pdr = [0]

            def load_expert(e):
                k = e % 2
                DMA("pool", wgu[k][:, :, 0:256], wg_d[l, e].rearrange("(kc p) c -> p kc c", p=128), [], [b_wgu[k]])
                DMA("pool", wgu[k][:, :, 256:512], wu_d[l, e].rearrange("(kc p) c -> p kc c", p=128), [], [b_wgu[k]])
                DMA("pool", wdn[k][:], wd_d[l, e].rearrange("(kc p) c -> p kc c", p=128), [], [b_wdn[k]])
                TT("pool", wdn[k][:], wdn[k][:], gbc[:, 1, :].unsqueeze(1).broadcast_to([128, 2, D]), ALU.mult, [b_wdn[k], b_gbc[1]], [b_wdn[k]])

            def gateup(e, tb):
                k = e % 2
                tok = slice(tb * 512, (tb + 1) * 512)
                CP("pool", sel32[:], identb[0:32, e:e + 1].broadcast_to([32, 128]), [b_identb], [b_sel32])
                pa, bpa = nextA()
                MM(pa[:], sel32[:], combT[:, tok], True, True, [b_sel32, b_combT[tb]], [bpa])
                CP("act", cbs[:], pa[:], [bpa], [b_cbs])
                hi_ = hidrr[0] % 2
                hidrr[0] += 1
                hid, b_hid = hids[hi_], b_hids[hi_]
                for hc in range(2):
                    pg, bpg = nextA()
                    for kc in range(8):
                        MM(pg[:], wgu[k][:, kc, hc * 128:(hc + 1) * 128], hTf[:, kc, tok], kc == 0, kc == 7, [b_wgu[k], b_hTf[tb]], [bpg])
                    pu, bpu = nextT()
                    for kc in range(8):
                        MM(pu[:], wgu[k][:, kc, 256 + hc * 128:256 + (hc + 1) * 128], hTf[:, kc, tok], kc == 0, kc == 7, [b_wgu[k], b_hTf[tb]], [bpu])
                    ACT(sgs[:], pg[:], AF.Silu, [bpg], [b_sgs])
                    TT("dve", tmo[:], sgs[:], pu[:], ALU.mult, [b_sgs, bpu], [b_tmo])
                    TT("pool", hid[:, hc, :], tmo[:], cbs[:], ALU.mult, [b_tmo, b_cbs], [b_hid])
                return hi_

            def down(e, tb, hi_):
                k = e % 2
                hid, b_hid = hids[hi_], b_hids[hi_]
                for tt in range(4):
                    i = tb * 4 + tt
                    for half in range(2):
                        pd, bpd = ((pSC[:, :], b_pSC), (pO[:, 0:512], b_pO), (pO[:, 512:1024], b_pO_hi))[pdr[0] % 3]
                        pdr[0] += 1
                        for hc in range(2):
                            MM(pd, hid[:, hc, tt * 128:(tt + 1) * 128], wdn[k][:, hc, half * 512:(half + 1) * 512], hc == 0, hc == 1,
                               [b_hid, b_wdn[k]], [bpd])
                        xs_ = X[:, i, half * 512:(half + 1) * 512]
                        TT("dve", xs_, xs_, pd, ALU.add, [bX[i], bpd], [bX[i]])

            pending = None
            for e in range(32):
                load_expert(e)
                for tb in range(4):
                    hi_ = gateup(e, tb)
                    if pending is not None:
                        down(*pending)
                    pending = (e, tb, hi_)
            down(*pending)

        convert_win(0)
        for l in range(n_layers):
            if stage < 1:
                break
            layer_params(l)
            barrier()
            if stage < 2:
                break
            if stage == 2.5:
                reset_states()
                break
            if stage not in (2, 2.2, 2.4):
                reset_states()
            halo_exchange(l)
            if stage in (2, 2.2, 2.4):
                break
            if stage < 3:
                break
            for blk in range(NBLK if CUT > 10 else 1):
                mixer_block(l, blk, False)
            if stage < 4:
                break
            state_exchange()
            DMA("pool", wout[:, :, :], wout_d[l].rearrange("(kc p) c -> p kc c", p=128), [], [b_wgu[0], b_wgu[1]])
            if stage < 5:
                break
            for blk in range(NBLK):
                mixer_block(l, blk, True)
            barrier()
            if stage < 6:
                break
            if l + 1 < n_layers:
                convert_win(l + 1)
            moe(l)
        finals = []
        barrier()
        if do_final:
            DMA("sp", gbc[:, 0, :], fng_d.broadcast_to([128, D]), [], [b_gbc[0]])
            for i in range(NT):
                ACT(yy[:, 0:D], X[:, i, :], AF.Square, [bX[i]], [b_yy, b_ssq], accum=ssq[:, 0:1])
                rstd_from_ssq(0, 1, 1.0 / D)
                STT(yy[:, 0:D], X[:, i, :], ssq[:, 0:1], gbc[:, 0, :], ALU.mult, ALU.mult, [bX[i], b_ssq, b_gbc[0]], [b_yy])
                finals.append(DMA("sp", out_d[i * 128:(i + 1) * 128, :], yy[:, 0:D], [b_yy], [S.buf()]))
        else:
            for i in range(NT):
                finals.append(DMA("sp", out_d[i * 128:(i + 1) * 128, :], X[:, i, :], [bX[i]], [S.buf()]))
        S.emit(final_waits=finals)
    return nc


def _prep_inputs(inp):
    f = lambda a: np.ascontiguousarray(np.asarray(a, dtype=np.float32))
    w_in = f(inp["w_in"])
    o = {}
    off = 0
    for name, w in (("z", 512), ("xs", 512), ("B", 128), ("C", 128), ("dt", 8), ("hq", 256), ("hf", 256), ("hi", 256),
                    ("hg", 256), ("rq", 256), ("rk", 256), ("rv", 256), ("rg", 256)):
        o[name] = (off, off + w)
        off += w
    sl = lambda n: w_in[:, :, o[n][0]:o[n][1]]

    def swap(a):
        a4 = a.reshape(a.shape[0], a.shape[1], 4, 2, 32)
        return np.ascontiguousarray(a4[:, :, :, ::-1, :]).reshape(a.shape)
    w_in_p = np.concatenate([sl("xs"), sl("B"), sl("C"), sl("hf"), sl("rk"), swap(sl("rk")), sl("hi"), sl("rv"), sl("dt"),
                             sl("hq"), sl("rq"), swap(sl("rq")), sl("z"), sl("hg"), sl("rg")], axis=2)
    assert w_in_p.shape[2] == DINP
    conv_w = f(inp["conv_w"]); conv_b = f(inp["conv_b"]); lbr = f(inp["hgrn_lower_bounds"])
    pm = np.zeros((DEPTH, NPM, 128), np.float32)
    for l in range(DEPTH):
        pm[l, 0:8] = f(inp["norm_mix_g"])[l].reshape(8, 128)
        pm[l, 8:16] = f(inp["norm_ffn_g"])[l].reshape(8, 128)
        for c in range(6):
            for j in range(4):
                pm[l, 16 + c * 4 + j] = conv_w[l, j, c * 128:(c + 1) * 128]
            pm[l, 40 + c] = conv_b[l, c * 128:(c + 1) * 128]
        for lp in range(DEPTH):
            for c in range(2):
                pm[l, 46 + 2 * lp + c] = lbr[lp, c * 128:(c + 1) * 128]
    bcrow = np.concatenate([f(inp["ssd_norm_g"]), f(inp["hgrn_norm_g"]), f(inp["ssd_d"]), f(inp["b_grp"]), f(inp["b_exp"])], axis=1)
    dtp = np.stack([f(inp["ssd_dt_bias"]), f(inp["ssd_a_log"])], axis=2)
    w_r = np.concatenate([f(inp["w_grp"]), f(inp["w_exp"])], axis=2)
    shared = {
        "pm": pm, "bcrow": np.ascontiguousarray(bcrow), "dtp": np.ascontiguousarray(dtp),
        "final_g": f(inp["final_norm_g"]).reshape(1, D), "w_ada": f(inp["w_ada"]), "b_ada": f(inp["b_ada"]),
        "w_in": np.ascontiguousarray(w_in_p), "w_out": f(inp["w_out"]), "w_r": np.ascontiguousarray(w_r),
        "w_gate": f(inp["w_gate"]), "w_up": f(inp["w_up"]), "w_down": f(inp["w_down"]),
    }
    x = f(inp["x"]); c = f(inp["c"])
    maps = []
    for core in range(NCORE):
        b, seg = core // 4, core % 4
        m = dict(shared)
        m["x"] = np.ascontiguousarray(x[b, seg * SEG:(seg + 1) * SEG, :])
        m["cT"] = np.ascontiguousarray(c[b].reshape(8, 128).T)
        sm = np.zeros((128, 4), np.float32); sm[:, :seg] = 1.0
        pv = np.zeros((128, 4), np.float32)
        if seg > 0:
            pv[:, seg - 1] = 1.0
        m["segmask"] = sm
        m["prevsel"] = pv
        for k, v in _consts(seg).items():
            m["c_" + k] = np.ascontiguousarray(v.astype(np.float32))
        maps.append(m)
    return maps


def kernel(**inputs):
    maps = _prep_inputs(inputs)
    nc = build()
    res = run_bass_kernel_spmd(nc, maps, core_ids=list(range(NCORE)))
    out = np.zeros((2, 4 * SEG, D), np.float32)
    for core in range(NCORE):
        b, seg = core // 4, core % 4
        out[b, seg * SEG:(seg + 1) * SEG, :] = np.asarray(res.results[core]["out"], dtype=np.float32)
    return out
```
